# Optimizing a Trainium2 kernel written in Bass

```python
import jax, jax.numpy as jnp
from jax import lax
import numpy as np

D_MODEL = 1024
BATCH = 16
SEQ = 2048
DEPTH = 2

HEAD_DIM = 64
D_MIX = D_MODEL
RET_HEADS = 4
GMLP_GROUPS = 4
NSA_HEADS = 8
NSA_KV_HEADS = 2
NSA_GROUP = NSA_HEADS // NSA_KV_HEADS
RET_WIDTH = RET_HEADS * HEAD_DIM
GMLP_WIDTH = GMLP_GROUPS * HEAD_DIM
NSA_WIDTH = NSA_HEADS * HEAD_DIM
KV_WIDTH = NSA_KV_HEADS * HEAD_DIM
N_BRANCH = 3
RET_CHUNK = 128
GMLP_CHUNK = 128
CMP_LEN = 32
CMP_STRIDE = 16
CMP_HIDDEN = 128
SEL_BLOCK = 64
SEL_TOPK = 8
WINDOW = 512
NSA_Q_BLOCK = 64
ROPE_THETA = 10000.0
N_EXPERTS = 16
N_EXPERT_GROUPS = 4
EXPERTS_PER_GROUP = N_EXPERTS // N_EXPERT_GROUPS
EXPERT_TOPK = 2
D_EXPERT = 512
NORM_EPS = 1e-6
NEG_INF = -1e30
FORCE_SCORE = 1e4
IN_SPLITS = (RET_WIDTH, RET_WIDTH, RET_WIDTH, RET_WIDTH, GMLP_WIDTH, GMLP_WIDTH, NSA_WIDTH,
             KV_WIDTH, KV_WIDTH, KV_WIDTH, KV_WIDTH, KV_WIDTH, KV_WIDTH, NSA_HEADS * N_BRANCH)
N_IN = sum(IN_SPLITS)

kernel_name = "hybrid_retention_gmlp_nsa_grouped_moe"


def rms_norm(x, g):
    xf = x.astype(jnp.float32)
    y = xf * lax.rsqrt(jnp.mean(xf * xf, axis=-1, keepdims=True) + NORM_EPS)
    return (y * g.astype(jnp.float32)).astype(x.dtype)


def layer_norm(x, g):
    xf = x.astype(jnp.float32)
    mu = jnp.mean(xf, axis=-1, keepdims=True)
    var = jnp.mean(jnp.square(xf - mu), axis=-1, keepdims=True)
    return ((xf - mu) * lax.rsqrt(var + NORM_EPS) * g.astype(jnp.float32)).astype(x.dtype)


def rope(x, pos):
    half = x.shape[-1] // 2
    inv_freq = jnp.power(ROPE_THETA, -jnp.arange(half, dtype=jnp.float32) / half)
    ang = pos.astype(jnp.float32)[..., None] * inv_freq
    cos = jnp.cos(ang)[:, :, None, :]
    sin = jnp.sin(ang)[:, :, None, :]
    xf = x.astype(jnp.float32)
    x1, x2 = xf[..., :half], xf[..., half:]
    return jnp.concatenate([x1 * cos - x2 * sin, x1 * sin + x2 * cos], axis=-1).astype(x.dtype)


def retention(q, k, v, g, pos, norm_g):
    B, S, H, d = q.shape
    C = RET_CHUNK
    nC = S // C
    dt = q.dtype
    q = rope(q, pos)
    k = rope(k, pos) * (d ** -0.5)
    log_gamma = jnp.log1p(-jnp.power(2.0, -5.0 - jnp.arange(H, dtype=jnp.float32)))
    idx = jnp.arange(C, dtype=jnp.float32)
    diff = idx[:, None] - idx[None, :]
    decay_in = jnp.where(diff >= 0, jnp.exp(jnp.maximum(diff, 0.0)[None] * log_gamma[:, None, None]), 0.0).astype(dt)
    decay_q = jnp.exp((idx + 1.0)[:, None] * log_gamma[None]).astype(dt)
    decay_k = jnp.exp((C - 1.0 - idx)[:, None] * log_gamma[None]).astype(dt)
    decay_chunk = jnp.exp(C * log_gamma).astype(dt)
    qc = q.reshape(B, nC, C, H, d).swapaxes(0, 1)
    kc = k.reshape(B, nC, C, H, d).swapaxes(0, 1)
    vc = v.reshape(B, nC, C, H, d).swapaxes(0, 1)

    def step(state, qkv):
        qb, kb, vb = qkv
        inner = jnp.einsum('bnhd,bmhd->bhnm', qb, kb) * decay_in
        o = jnp.einsum('bhnm,bmhe->bnhe', inner, vb)
        o = o + jnp.einsum('bnhd,bhde->bnhe', qb, state) * decay_q[None, :, :, None]
        state = state * decay_chunk[None, :, None, None] + jnp.einsum('bmhd,bmhe->bhde', kb * decay_k[None, :, :, None], vb)
        return state, o

    state0 = jnp.zeros((B, H, d, d), dt)
    _, o = lax.scan(step, state0, (qc, kc, vc))
    o = rms_norm(o.swapaxes(0, 1).reshape(B, S, H, d), norm_g)
    return o.reshape(B, S, H * d) * jax.nn.silu(g)


def chunked_spatial_gating(u, v, ln_g, w_s, b_s):
    B, S, _ = u.shape
    nC = S // GMLP_CHUNK
    u = jax.nn.gelu(u)
    v = layer_norm(jax.nn.gelu(v), ln_g)
    vc = v.reshape(B, nC, GMLP_CHUNK, GMLP_GROUPS, HEAD_DIM)
    causal = jnp.tril(jnp.ones((GMLP_CHUNK, GMLP_CHUNK), dtype=bool))
    w = jnp.where(causal[None], w_s, 0.0)
    s = jnp.einsum('gts,bnsgc->bntgc', w, vc) + b_s.T[None, None, :, :, None]
    return u * s.reshape(B, S, GMLP_WIDTH)


def compress_blocks(kv, pe, w1, w2):
    B, S, G, d = kv.shape
    chunks = kv.reshape(B, S // CMP_STRIDE, CMP_STRIDE, G, d)
    blocks = jnp.concatenate([chunks[:, :-1], chunks[:, 1:]], axis=2)
    h = jax.nn.gelu(jnp.einsum('bnlgd,ldh->bngh', blocks + pe[None, None, :, None, :], w1))
    return jnp.einsum('bngh,hd->bngd', h, w2)


def masked_softmax(s, mask):
    p = jax.nn.softmax(jnp.where(mask, s.astype(jnp.float32), NEG_INF), axis=-1)
    return p * mask


def native_sparse_attention(q, kc_raw, vc_raw, ks, vs, kw, vw, gate_logits, pos,
                            q_norm_g, k_norm_g, pe_k, pe_v, w1_k, w2_k, w1_v, w2_v):
    B, S, H, d = q.shape
    G, R = NSA_KV_HEADS, NSA_GROUP
    dt = q.dtype
    scale = d ** -0.5
    q = rope(rms_norm(q, q_norm_g), pos)
    kc = compress_blocks(kc_raw, pe_k, w1_k, w2_k)
    vc = compress_blocks(vc_raw, pe_v, w1_v, w2_v)
    kc = rope(rms_norm(kc, k_norm_g[0]), pos[:, CMP_LEN - 1::CMP_STRIDE])
    ks = rope(rms_norm(ks, k_norm_g[1]), pos)
    kw = rope(rms_norm(kw, k_norm_g[2]), pos)
    n_cmp = kc.shape[1]
    n_sel = S // SEL_BLOCK
    top_n = min(SEL_TOPK, n_sel)
    cmp_start = jnp.arange(n_cmp) * CMP_STRIDE
    cmp_end = cmp_start + CMP_LEN - 1
    sel_start = jnp.arange(n_sel) * SEL_BLOCK
    overlap = ((cmp_start[:, None] < sel_start[None] + SEL_BLOCK) & (cmp_start[:, None] + CMP_LEN > sel_start[None])).astype(jnp.float32)
    ks_blk = ks.reshape(B, n_sel, SEL_BLOCK, G, d).transpose(0, 3, 1, 2, 4)
    vs_blk = vs.reshape(B, n_sel, SEL_BLOCK, G, d).transpose(0, 3, 1, 2, 4)
    kw_pad = jnp.pad(kw, ((0, 0), (WINDOW, 0), (0, 0), (0, 0)))
    vw_pad = jnp.pad(vw, ((0, 0), (WINDOW, 0), (0, 0), (0, 0)))
    gates = jax.nn.sigmoid(gate_logits.reshape(B, S, G, R, N_BRANCH))
    n_blk = S // NSA_Q_BLOCK
    q_blocks = q.reshape(B, n_blk, NSA_Q_BLOCK, G, R, d).swapaxes(0, 1)
    g_blocks = gates.reshape(B, n_blk, NSA_Q_BLOCK, G, R, N_BRANCH).swapaxes(0, 1)
    b_ix = jnp.arange(B)[:, None, None, None]
    g_ix = jnp.arange(G)[None, :, None, None]
    sel_off = jnp.arange(SEL_BLOCK)
    win_off = jnp.arange(NSA_Q_BLOCK + WINDOW) - WINDOW
    j = jnp.arange(n_sel)

    def block(args):
        i, qb, gb = args
        t = i * NSA_Q_BLOCK + jnp.arange(NSA_Q_BLOCK)
        s_c = jnp.einsum('bqgrd,bngd->bgrqn', qb, kc) * scale
        p_c = masked_softmax(s_c, cmp_end[None, :] <= t[:, None])
        o_c = jnp.einsum('bgrqn,bngd->bqgrd', p_c.astype(dt), vc)
        imp = jnp.einsum('bgrqn,nj->bgqj', p_c, overlap)
        cur = t // SEL_BLOCK
        forced = (j[None] == 0) | (j[None] == cur[:, None]) | (j[None] == cur[:, None] - 1)
        allowed = sel_start[None] <= t[:, None]
        score = jnp.where(forced, FORCE_SCORE, jnp.where(allowed, imp, -1.0))
        _, idx = lax.top_k(score, top_n)
        k_g = ks_blk[b_ix, g_ix, idx]
        v_g = vs_blk[b_ix, g_ix, idx]
        tok = idx[..., None] * SEL_BLOCK + sel_off
        m_s = (tok <= t[None, None, :, None, None]).reshape(B, G, 1, NSA_Q_BLOCK, top_n * SEL_BLOCK)
        s_s = jnp.einsum('bqgrd,bgqkld->bgrqkl', qb, k_g).reshape(B, G, R, NSA_Q_BLOCK, top_n * SEL_BLOCK) * scale
        p_s = masked_softmax(s_s, m_s).reshape(B, G, R, NSA_Q_BLOCK, top_n, SEL_BLOCK)
        o_s = jnp.einsum('bgrqkl,bgqkld->bqgrd', p_s.astype(dt), v_g)
        start = i * NSA_Q_BLOCK
        k_w = lax.dynamic_slice_in_dim(kw_pad, start, NSA_Q_BLOCK + WINDOW, axis=1)
        v_w = lax.dynamic_slice_in_dim(vw_pad, start, NSA_Q_BLOCK + WINDOW, axis=1)
        spos = start + win_off
        m_w = (spos[None] <= t[:, None]) & (t[:, None] - spos[None] < WINDOW) & (spos[None] >= 0)
        s_w = jnp.einsum('bqgrd,bsgd->bgrqs', qb, k_w) * scale
        p_w = masked_softmax(s_w, m_w)
        o_w = jnp.einsum('bgrqs,bsgd->bqgrd', p_w.astype(dt), v_w)
        return gb[..., 0:1] * o_c + gb[..., 1:2] * o_s + gb[..., 2:3] * o_w

    out = lax.map(block, (jnp.arange(n_blk), q_blocks, g_blocks))
    return out.swapaxes(0, 1).reshape(B, S, H * d)


def grouped_moe(h, router_w, router_b, w_gate, w_up, w_down):
    B, S, D = h.shape
    probs = jax.nn.softmax((h @ router_w).astype(jnp.float32), axis=-1)
    sel = (probs + router_b.astype(jnp.float32)).reshape(B, S, N_EXPERT_GROUPS, EXPERTS_PER_GROUP)
    group_score = jnp.sum(lax.top_k(sel, EXPERT_TOPK)[0], axis=-1)
    best_group = jnp.argmax(group_score, axis=-1)
    in_group = (best_group[..., None] == jnp.arange(N_EXPERT_GROUPS))[..., None]
    sel = jnp.where(in_group, sel, NEG_INF).reshape(B, S, N_EXPERTS)
    _, top_idx = lax.top_k(sel, EXPERT_TOPK)
    top_w = jnp.take_along_axis(probs, top_idx, axis=-1)
    top_w = top_w / jnp.sum(top_w, axis=-1, keepdims=True)
    gates = jnp.einsum('bske,bsk->bse', jax.nn.one_hot(top_idx, N_EXPERTS, dtype=jnp.float32), top_w).astype(h.dtype)
    out = jnp.zeros_like(h)
    for e in range(N_EXPERTS):
        a = jax.nn.silu(h @ w_gate[e]) * (h @ w_up[e])
        out = out + (gates[..., e:e + 1] * a) @ w_down[e]
    return out


def setup_inputs(seed: int = 0) -> dict:
    key = jax.random.key(seed)
    ks = jax.random.split(key, 32)
    f32 = jnp.float32
    nrm = lambda k, shape, s: jax.random.normal(k, shape, f32) * s
    L = DEPTH
    return {
        "x": nrm(ks[0], (BATCH, SEQ, D_MODEL), 1.0),
        "c": nrm(ks[1], (BATCH, D_MODEL), 1.0),
        "positions": (jnp.arange(SEQ, dtype=jnp.int32)[None, :] + jax.random.randint(ks[2], (BATCH, 1), 0, 1024, dtype=jnp.int32)),
        "ada_w": nrm(ks[3], (L, D_MODEL, 6 * D_MODEL), 0.5 * D_MODEL ** -0.5),
        "ada_b": nrm(ks[4], (L, 6 * D_MODEL), 0.02),
        "norm_mix_g": 1.0 + nrm(ks[5], (L, D_MODEL), 0.05),
        "norm_ffn_g": 1.0 + nrm(ks[6], (L, D_MODEL), 0.05),
        "w_in": nrm(ks[7], (L, D_MODEL, N_IN), D_MODEL ** -0.5),
        "w_out": nrm(ks[8], (L, D_MIX, D_MODEL), D_MIX ** -0.5),
        "ret_norm_g": 1.0 + nrm(ks[9], (L, RET_HEADS, HEAD_DIM), 0.05),
        "gmlp_ln_g": 1.0 + nrm(ks[10], (L, GMLP_WIDTH), 0.05),
        "gmlp_ws": nrm(ks[11], (L, GMLP_GROUPS, GMLP_CHUNK, GMLP_CHUNK), GMLP_CHUNK ** -0.5),
        "gmlp_b": 1.0 + nrm(ks[12], (L, GMLP_GROUPS, GMLP_CHUNK), 0.05),
        "nsa_q_norm_g": 1.0 + nrm(ks[13], (L, HEAD_DIM), 0.05),
        "nsa_k_norm_g": 1.0 + nrm(ks[14], (L, N_BRANCH, HEAD_DIM), 0.05),
        "cmp_pe_k": nrm(ks[15], (L, CMP_LEN, HEAD_DIM), 0.1),
        "cmp_pe_v": nrm(ks[16], (L, CMP_LEN, HEAD_DIM), 0.1),
        "cmp_w1_k": nrm(ks[17], (L, CMP_LEN, HEAD_DIM, CMP_HIDDEN), (CMP_LEN * HEAD_DIM) ** -0.5),
        "cmp_w2_k": nrm(ks[18], (L, CMP_HIDDEN, HEAD_DIM), CMP_HIDDEN ** -0.5),
        "cmp_w1_v": nrm(ks[19], (L, CMP_LEN, HEAD_DIM, CMP_HIDDEN), (CMP_LEN * HEAD_DIM) ** -0.5),
        "cmp_w2_v": nrm(ks[20], (L, CMP_HIDDEN, HEAD_DIM), CMP_HIDDEN ** -0.5),
        "router_w": nrm(ks[21], (D_MODEL, N_EXPERTS), D_MODEL ** -0.5),
        "router_b": nrm(ks[22], (N_EXPERTS,), 0.01),
        "moe_w_gate": nrm(ks[23], (L, N_EXPERTS, D_MODEL, D_EXPERT), D_MODEL ** -0.5),
        "moe_w_up": nrm(ks[24], (L, N_EXPERTS, D_MODEL, D_EXPERT), D_MODEL ** -0.5),
        "moe_w_down": nrm(ks[25], (L, N_EXPERTS, D_EXPERT, D_MODEL), D_EXPERT ** -0.5),
    }


def reference(x, c, positions, ada_w, ada_b, norm_mix_g, norm_ffn_g, w_in, w_out,
              ret_norm_g, gmlp_ln_g, gmlp_ws, gmlp_b, nsa_q_norm_g, nsa_k_norm_g,
              cmp_pe_k, cmp_pe_v, cmp_w1_k, cmp_w2_k, cmp_w1_v, cmp_w2_v,
              router_w, router_b, moe_w_gate, moe_w_up, moe_w_down):
    B, S, _ = x.shape
    offsets = [int(o) for o in np.cumsum(IN_SPLITS)[:-1]]
    for l in range(DEPTH):
        mod = jax.nn.silu(c) @ ada_w[l] + ada_b[l]
        sh1, sc1, g1, sh2, sc2, g2 = [m[:, None, :] for m in jnp.split(mod, 6, axis=-1)]
        h = rms_norm(x, norm_mix_g[l]) * (1.0 + sc1) + sh1
        z = h @ w_in[l]
        rq, rk, rv, rg, gu, gv, nq, kc, vc, kse, vse, kwi, vwi, gl = jnp.split(z, offsets, axis=-1)
        head = lambda a, n: a.reshape(B, S, n, HEAD_DIM)
        y_ret = retention(head(rq, RET_HEADS), head(rk, RET_HEADS), head(rv, RET_HEADS), rg, positions, ret_norm_g[l])
        y_gmlp = chunked_spatial_gating(gu, gv, gmlp_ln_g[l], gmlp_ws[l], gmlp_b[l])
        y_nsa = native_sparse_attention(
            head(nq, NSA_HEADS), head(kc, NSA_KV_HEADS), head(vc, NSA_KV_HEADS),
            head(kse, NSA_KV_HEADS), head(vse, NSA_KV_HEADS), head(kwi, NSA_KV_HEADS), head(vwi, NSA_KV_HEADS),
            gl, positions, nsa_q_norm_g[l], nsa_k_norm_g[l],
            cmp_pe_k[l], cmp_pe_v[l], cmp_w1_k[l], cmp_w2_k[l], cmp_w1_v[l], cmp_w2_v[l])
        y = jnp.concatenate([y_ret, y_gmlp, y_nsa], axis=-1) @ w_out[l]
        x = x + g1 * y
        h2 = rms_norm(x, norm_ffn_g[l]) * (1.0 + sc2) + sh2
        x = x + g2 * grouped_moe(h2, router_w, router_b, moe_w_gate[l], moe_w_up[l], moe_w_down[l])
    return x
```

```python
import numpy as np
from contextlib import ExitStack
import ml_dtypes
import concourse.bass as bass
import concourse.mybir as mybir
from concourse.bass_utils import run_bass_kernel_spmd

F32 = mybir.dt.float32
BF16 = mybir.dt.bfloat16
I32 = mybir.dt.int32
AF = mybir.ActivationFunctionType
ALU = mybir.AluOpType
AX = mybir.AxisListType

S = 2048
DM = 1024
NT = 16
NE = 16
EPS = 1e-6
NEG = -30000.0
PI = float(np.pi)
TWO_PI = float(2 * np.pi)
N_IN = 2840
import os
SAME_ENGINE_SYNC = os.environ.get("K_SES", "1") == "1"


class _Rec:
    def __init__(self):
        self.call = None

    def __getattr__(self, name):
        def f(*a, **k):
            assert self.call is None
            self.call = (name, a, k)
            return self
        return f


def _bind(fn):
    rec = _Rec()
    fn(rec)
    name, a, k = rec.call
    return lambda e: getattr(e, name)(*a, **k)


class Res:
    __slots__ = ("w", "r")

    def __init__(self):
        self.w = None
        self.r = {}


class FW:
    ENGS = ("tensor", "vector", "scalar", "gpsimd", "sync")

    def __init__(self, nc, stack):
        self.nc = nc
        self.stack = stack
        self.ops = {e: [] for e in self.ENGS}
        self.sem = {}
        self.cnt = {}
        for e in self.ENGS:
            self.sem[e] = stack.enter_context(nc.semaphore("s_" + e))
            self.cnt[e] = 0
        self.seen = {e: {} for e in self.ENGS}
        self.dsems = []

    def dma_sem(self, name):
        s = self.stack.enter_context(self.nc.semaphore(name))
        d = {"sem": s, "cnt": 0, "key": "d_" + name}
        self.dsems.append(d)
        return d

    def _deps(self, eng, reads, writes):
        deps = {}

        def add(t):
            if t is None:
                return
            key, sem, val = t
            if key == eng and (eng == "tensor" or not SAME_ENGINE_SYNC):
                return
            if deps.get(key, (None, -1))[1] < val:
                deps[key] = (sem, val)

        for r in reads:
            add(r.w)
        for w in writes:
            add(w.w)
            for t in w.r.values():
                add(t)
        out = []
        for key, (sem, val) in deps.items():
            if self.seen[eng].get(key, -1) >= val:
                continue
            self.seen[eng][key] = val
            out.append((sem, val))
        return out

    def _mark(self, tok, reads, writes):
        for r in reads:
            r.r[tok[0]] = tok
        for w in writes:
            w.w = tok
            w.r = {}

    def op(self, eng, fn, reads=(), writes=()):
        waits = self._deps(eng, reads, writes)
        self.cnt[eng] += 1
        tok = (eng, self.sem[eng], self.cnt[eng])
        self._mark(tok, reads, writes)
        self.ops[eng].append((waits, _bind(fn), self.sem[eng], 1))
        return tok

    def dma(self, eng, dsem, out, in_, reads=(), writes=()):
        waits = self._deps(eng, reads, writes)
        dsem["cnt"] += 16
        tok = (dsem["key"], dsem["sem"], dsem["cnt"])
        self._mark(tok, reads, writes)
        self.ops[eng].append((waits, lambda e: e.dma_start(out=out, in_=in_), dsem["sem"], 16))
        return tok

    def group_end(self, dsem, resources):
        tok = (dsem["key"], dsem["sem"], dsem["cnt"])
        for r in resources:
            r.w = tok

    def barrier(self):
        toks = [(e, self.sem[e], self.cnt[e]) for e in self.ENGS if self.cnt[e] > 0]
        toks += [(d["key"], d["sem"], d["cnt"]) for d in self.dsems if d["cnt"] > 0]
        for eng in self.ENGS:
            waits = []
            for key, sem, val in toks:
                if key == eng:
                    continue
                if self.seen[eng].get(key, -1) >= val:
                    continue
                self.seen[eng][key] = val
                waits.append((sem, val))
            if waits:
                self.ops[eng].append((waits, None, None, 0))

    def build(self):
        nc = self.nc
        with nc.Block() as block:
            def mk(name):
                def body(e):
                    for waits, fn, sem, inc in self.ops[name]:
                        for (s, v) in waits:
                            e.wait_ge(s, v)
                        if fn is not None:
                            fn(e).then_inc(sem, inc)
                return body
            block.tensor(mk("tensor"))
            block.vector(mk("vector"))
            block.scalar(mk("scalar"))
            block.gpsimd(mk("gpsimd"))
            block.sync(mk("sync"))


class _Stop(Exception):
    pass


class Ring:
    def __init__(self, bufs):
        self.bufs = [(b, Res()) for b in bufs]
        self.i = 0

    def next(self):
        b = self.bufs[self.i % len(self.bufs)]
        self.i += 1
        return b


def _consts():
    f32 = np.float32
    c = {}
    c["ident_f"] = np.eye(128, dtype=f32)
    half = 32
    inv_freq = np.power(f32(10000.0), -np.arange(half, dtype=f32) / f32(half)).astype(f32)
    c["invf"] = np.tile(inv_freq[None, :], (128, 1)).astype(f32)
    t = np.arange(128)
    c["tril"] = (t[None, :] <= t[:, None]).astype(f32)
    H = 4
    log_gamma = np.log1p(-np.power(2.0, -5.0 - np.arange(H))).astype(np.float64)
    diff = t[None, :] - t[:, None]
    decT = np.zeros((128, H, 128), dtype=f32)
    for h in range(H):
        decT[:, h, :] = np.where(diff >= 0, np.exp(np.maximum(diff, 0) * log_gamma[h]), 0.0)
    c["decT"] = decT.reshape(128, H * 128)
    c["decq"] = np.exp((t[:, None] + 1.0) * log_gamma[None, :]).astype(f32)
    c["deck"] = np.exp((127.0 - t[:, None]) * log_gamma[None, :]).astype(f32)
    dchunk = [float(np.exp(128.0 * log_gamma[h])) for h in range(H)]
    tt = np.arange(S)
    j = np.arange(32)
    cur = tt // 64
    forced = (j[None, :] == 0) | (j[None, :] == cur[:, None]) | (j[None, :] == cur[:, None] - 1)
    allowed = (j[None, :] * 64) <= tt[:, None]
    a01 = (allowed & ~forced).astype(f32)
    addc = np.where(forced, 1e4, np.where(allowed, 0.0, -1.0)).astype(f32)
    c["a01"] = a01.reshape(NT, 128, 32).transpose(1, 0, 2).reshape(128, NT * 32)
    c["addc"] = addc.reshape(NT, 128, 32).transpose(1, 0, 2).reshape(128, NT * 32)
    ones = np.ones((128, 128), dtype=f32)
    c["ones_f"] = ones
    cf_names = ["ident_f", "invf", "tril", "decT", "decq", "deck", "a01", "addc", "ones_f"]
    cf_off = {}
    o = 0
    for n in cf_names:
        cf_off[n] = (o, c[n].shape[1])
        o += c[n].shape[1]
    cf = np.concatenate([c[n] for n in cf_names], axis=1).astype(f32)

    b = {}
    b["ident_b"] = np.eye(128, dtype=f32)
    key = t[:, None]
    q = t[None, :]
    b["causT"] = np.where(key <= q, 0.0, NEG).astype(f32)
    b["antiT"] = np.where(key > q, 0.0, NEG).astype(f32)
    n = np.arange(128)
    cm = np.where((16 * n[:, None] + 31) <= tt[None, :], 0.0, NEG).astype(f32)
    cm[127, :] = NEG
    b["cmaskT"] = cm
    E = np.zeros((128, NT, 128), dtype=f32)
    for jj in range(NT):
        for kk in range(128):
            E[2 * jj + kk // 64, jj, kk] = -NEG
    b["Esel"] = E.reshape(128, NT * 128)
    ov = np.zeros((128, 32), dtype=f32)
    for nn in range(127):
        for jj in range(32):
            if (16 * nn < 64 * jj + 64) and (16 * nn + 32 > 64 * jj):
                ov[nn, jj] = 1.0
    b["ovl"] = ov
    b["ones_b"] = np.ones((128, 128), dtype=f32)
    cb_names = ["ident_b", "causT", "antiT", "cmaskT", "Esel", "ovl", "ones_b"]
    cb_off = {}
    o = 0
    for nme in cb_names:
        cb_off[nme] = (o, b[nme].shape[1])
        o += b[nme].shape[1]
    cb = np.concatenate([b[nme] for nme in cb_names], axis=1).astype(ml_dtypes.bfloat16)
    return cf, cf_off, cb, cb_off, dchunk


_CF, _CF_OFF, _CB, _CB_OFF, _DCHUNK = _consts()

O_RQ, O_RK, O_RV, O_RG, O_GU, O_GV, O_NQ = 0, 256, 512, 768, 1024, 1280, 1536
O_KC, O_VC, O_KS, O_VS, O_KW, O_VW, O_GL = 2048, 2176, 2304, 2432, 2560, 2688, 2816

WEIGHT_SPECS = [
    ("ada_w", [2, 1024, 6144]), ("ada_b", [2, 6144]), ("norm_mix_g", [2, 1024]),
    ("norm_ffn_g", [2, 1024]), ("w_in", [2, 1024, N_IN]), ("w_out", [2, 1024, 1024]),
    ("ret_norm_g", [2, 4, 64]), ("gmlp_ln_g", [2, 256]), ("gmlp_ws", [2, 4, 128, 128]),
    ("gmlp_b", [2, 4, 128]), ("nsa_q_norm_g", [2, 64]), ("nsa_k_norm_g", [2, 3, 64]),
    ("cmp_pe_k", [2, 32, 64]), ("cmp_pe_v", [2, 32, 64]), ("cmp_w1_k", [2, 32, 64, 128]),
    ("cmp_w2_k", [2, 128, 64]), ("cmp_w1_v", [2, 32, 64, 128]), ("cmp_w2_v", [2, 128, 64]),
    ("router_w", [1024, 16]), ("router_b", [16]), ("moe_w_gate", [2, 16, 1024, 512]),
    ("moe_w_up", [2, 16, 1024, 512]), ("moe_w_down", [2, 16, 512, 1024]),
]


def build_program(nseq=2, nlayers=2, dbg=None, stop_after=None):
    dbg = dbg or {}
    nc = bass.Bass("TRN2", target_bir_lowering=False)

    def D(name, shape, dt, kind="ExternalInput"):
        return nc.dram_tensor(name, shape, dt, kind=kind).ap()

    x_d = D("x", [nseq, S, DM], F32)
    c_d = D("c", [nseq, DM], F32)
    pos_d = D("positions", [nseq, S], I32)
    W = {n: D(n, shp, F32) for n, shp in WEIGHT_SPECS}
    cf_d = D("cf", list(_CF.shape), F32)
    cb_d = D("cb", list(_CB.shape), BF16)
    out_d = D("out", [nseq, S, DM], F32, kind="ExternalOutput")
    xscr_d = D("xscr", [S, DM], F32, kind="Internal")
    dbg_d = {k: D("dbg_" + k, shp, dt, kind="ExternalOutput") for k, (shp, dt) in dbg.items() if not k.startswith("_")}

    with ExitStack() as st:
        fw = FW(nc, st)
        st.enter_context(nc.allow_non_contiguous_dma(reason="small strided parameter loads"))

        ar = {"cur": 16512, "hi": 0}
        cnt = [0]

        def A(shape, dt, name=None):
            nb = int(np.prod(shape[1:])) * (4 if dt in (F32, I32) else 2)
            off = (ar["cur"] + 63) // 64 * 64
            ar["cur"] = off + nb
            ar["hi"] = max(ar["hi"], ar["cur"])
            assert ar["cur"] <= 229376, f"SBUF overflow {ar['cur']}"
            cnt[0] += 1
            return nc.alloc_sbuf_tensor_at(f"{name or 't'}_{cnt[0]}", list(shape), dt, offset=off)

        def mark():
            return ar["cur"]

        def release(m):
            fw.barrier()
            ar["cur"] = m

        pf = Ring([nc.alloc_psum_tensor(f"pf{i}", [128, 512], F32) for i in range(3)])
        pacc = Ring([nc.alloc_psum_tensor(f"pa{i}", [128, 512], F32) for i in range(3)])
        pfA = Ring([])
        pfA.bufs = pf.bufs[0:2]
        paccA = Ring([])
        paccA.bufs = [pf.bufs[2]] + pacc.bufs
        pb = Ring([nc.alloc_psum_tensor(f"pb{i}", [128, 8, 128], BF16) for i in range(2)])

        def V(fn, r=(), w=()):
            return fw.op("vector", fn, r, w)

        def SC(fn, r=(), w=()):
            return fw.op("scalar", fn, r, w)

        def T(fn, r=(), w=()):
            return fw.op("tensor", fn, r, w)

        def G(fn, r=(), w=()):
            return fw.op("gpsimd", fn, r, w)

        cf = A(list(_CF.shape), F32, "cf")
        cb = A(list(_CB.shape), BF16, "cb")
        r_c = Res()
        dconst = fw.dma_sem("dconst")
        fw.dma("sync", dconst, cf[:], cf_d, writes=[r_c])
        fw.dma("sync", dconst, cb[:], cb_d, writes=[r_c])

        def CF(name, rows=128):
            o, n = _CF_OFF[name]
            return cf[0:rows, o:o + n]

        def CB(name, rows=128):
            o, n = _CB_OFF[name]
            return cb[0:rows, o:o + n]

        ident_f = CF("ident_f")
        ident_b = CB("ident_b")

        modT = A([128, 2, 6, 8, nseq], F32, "modT")
        r_mod = Res()
        gmixT = A([128, 2, 2, 8], F32, "gT")
        r_g = Res()
        WTg = A([128, 2, 4, 128], BF16, "WTg")
        bTg = A([128, 2, 4], F32, "bTg")
        r_wtg = Res()
        gq_bc = A([128, 2, 64], F32, "gq")
        gk_bc = A([128, 2, 3, 64], F32, "gk")
        gret_bc = A([128, 2, 256], F32, "gret")
        gln_bc = A([128, 2, 256], F32, "gln")
        rb_bc = A([128, 16], F32, "rb")
        rw_sb = A([128, 8, 16], BF16, "rw")
        r_small = Res()
        dsm = fw.dma_sem("dsmall")
        for l in range(nlayers):
            fw.dma("sync", dsm, gq_bc[:, l, :], W["nsa_q_norm_g"][l].partition_broadcast(128), writes=[r_small])
            fw.dma("sync", dsm, gk_bc[:, l, :, :], W["nsa_k_norm_g"][l].partition_broadcast(128), writes=[r_small])
            fw.dma("sync", dsm, gret_bc[:, l, :], W["ret_norm_g"][l].rearrange("h d -> (h d)").partition_broadcast(128), writes=[r_small])
            fw.dma("sync", dsm, gln_bc[:, l, :], W["gmlp_ln_g"][l].partition_broadcast(128), writes=[r_small])
        fw.dma("sync", dsm, rb_bc[:], W["router_b"].partition_broadcast(128), writes=[r_small])
        drw = fw.dma_sem("drw")
        r_rw = Res()
        fw.dma("gpsimd", drw, rw_sb[:], W["router_w"].rearrange("(k p) e -> p k e", p=128), writes=[r_rw])
        fw.group_end(dconst, [r_c])
        fw.group_end(dsm, [r_small])
        V(lambda e: e.tensor_scalar(gq_bc[:], gq_bc[:], 0.125, None, op0=ALU.mult), [r_small], [r_small])

        m_setup = mark()
        stg = A([128, 512], F32, "stg")
        r_stg = Res()
        dstg = fw.dma_sem("dstg")

        def load_T(src_ap, rows, dst_fn, post=None):
            fw.dma("sync", dstg, stg[0:rows, 0:128], src_ap, writes=[r_stg])
            if post is not None:
                post(stg[0:rows, 0:128])
            pt, rp = pf.next()
            T(lambda e: e.transpose(pt[:, 0:rows], stg[0:rows, 0:128], ident_f[0:rows, 0:rows]), [r_stg, r_c], [rp])
            dst_fn(pt[:, 0:rows], rp)

        scT = A([128, 8, nseq], BF16, "scT")
        r_scT = Res()

        def post_silu(ap):
            SC(lambda e: e.activation(ap, ap, AF.Silu), [r_stg], [r_stg])

        def dst_sc(ps, rp):
            V(lambda e: e.tensor_copy(scT[:].rearrange("p k b -> p b k"), ps.rearrange("p (b k) -> p b k", k=8)), [rp], [r_scT])

        load_T(c_d.rearrange("b (k p) -> (b k) p", p=128), 8 * nseq, dst_sc, post_silu)

        adab = A([128, 48], F32, "adab")
        r_adab = Res()
        adaw = [A([128, 8, 768], BF16, f"adaw{i}") for i in range(2)]
        adaw_r = [Res(), Res()]
        adaw_d = [fw.dma_sem("dadaw0"), fw.dma_sem("dadaw1")]
        blk = 0
        for l in range(nlayers):
            def dst_b(ps, rp):
                V(lambda e: e.tensor_copy(adab[:], ps), [rp], [r_adab])
            load_T(W["ada_b"][l].rearrange("(j p) -> j p", p=128), 48, dst_b)
            for gi, gname in enumerate(["norm_mix_g", "norm_ffn_g"]):
                def dst_g(ps, rp, gi=gi, l=l):
                    V(lambda e: e.tensor_copy(gmixT[:, l, gi, :], ps), [rp], [r_g])
                load_T(W[gname][l].rearrange("(k p) -> k p", p=128), 8, dst_g)
            for jb in range(8):
                wb, wr, wd = adaw[blk % 2], adaw_r[blk % 2], adaw_d[blk % 2]
                blk += 1
                fw.dma("gpsimd", wd, wb[:], W["ada_w"][l, :, jb * 768:(jb + 1) * 768].rearrange("(k p) n -> p k n", p=128), writes=[wr])
                pt, rp = pf.next()
                for jj in range(6):
                    for k in range(8):
                        T(lambda e, jj=jj, k=k, wb=wb, pt=pt: e.matmul(pt[:, jj * nseq:(jj + 1) * nseq], wb[:, k, jj * 128:(jj + 1) * 128], scT[:, k, :], start=(k == 0), stop=(k == 7)),
                          [wr, r_scT], [rp])
                for jj in range(6):
                    j = jb * 6 + jj
                    V(lambda e, jj=jj, j=j, pt=pt, l=l: e.tensor_tensor(modT[:, l, j // 8, j % 8, :], pt[:, jj * nseq:(jj + 1) * nseq],
                                                               adab[:, j:j + 1].to_broadcast([128, nseq]), ALU.add), [rp, r_adab], [r_mod])
        for l in range(nlayers):
            for gi, wh in ((0, 1), (1, 4)):
                V(lambda e, l=l, gi=gi, wh=wh: e.scalar_tensor_tensor(modT[:, l, wh, :, :], modT[:, l, wh, :, :], 1.0,
                                                                       gmixT[:, l, gi, :].unsqueeze(2).to_broadcast([128, 8, nseq]),
                                                                       op0=ALU.add, op1=ALU.mult), [r_mod, r_g], [r_mod])

        for l in range(nlayers):
            wst = A([128, 4, 128], F32, "wst")
            r_wst = Res()
            dwst = fw.dma_sem(f"dwst{l}")
            fw.dma("sync", dwst, wst[:], W["gmlp_ws"][l].rearrange("g t s -> t g s"), writes=[r_wst])
            V(lambda e, wst=wst: e.tensor_tensor(wst[:], wst[:], CF("tril").unsqueeze(1).to_broadcast([128, 4, 128]), ALU.mult), [r_wst, r_c], [r_wst])
            pt, rp = pf.next()
            for g in range(4):
                T(lambda e, g=g, pt=pt, wst=wst: e.transpose(pt[:, g * 128:(g + 1) * 128], wst[:, g, :], ident_f), [r_wst, r_c], [rp])
            V(lambda e, pt=pt, l=l: e.tensor_copy(WTg[:, l, :, :], pt[:].rearrange("p (g t) -> p g t", g=4)), [rp], [r_wtg])

            def dst_bt(ps, rp, l=l):
                V(lambda e: e.tensor_copy(bTg[:, l, :], ps), [rp], [r_wtg])
            load_T(W["gmlp_b"][l], 4, dst_bt)
        release(m_setup)

        dbg_sem = fw.dma_sem("ddbg")
        out_toks = []

        def dump(name, ap, res_list):
            if name in dbg_d:
                t = fw.dma("sync", dbg_sem, dbg_d[name], ap, reads=res_list)
                rr = Res()
                rr.w = t
                out_toks.append(rr)

        dump("modT", modT[:], [r_mod])
        dump("WTg", WTg[:], [r_wtg])

        yT = A([128, 8, S], BF16, "yT")
        r_yT = [Res() for _ in range(NT)]
        wf = Ring([A([128, 512], F32, "wf") for _ in range(6)])
        wbf = Ring([A([128, 512], BF16, "wbf") for _ in range(3)])
        ws = Ring([A([128, 32], F32, "ws") for _ in range(10)])
        dx = [fw.dma_sem(f"dx{i}") for i in range(2)]
        dwb = [fw.dma_sem(f"dwb{i}") for i in range(2)]
        dmoe = [fw.dma_sem(f"dmoe{i}") for i in range(2)]
        dmisc = fw.dma_sem("dmisc")
        dout = fw.dma_sem("dout")
        m_seq = mark()

        def rope(src, dst, H, cs, sn, rs, rd, rows=128):
            x1 = src[:, :, 0:32]
            x2 = src[:, :, 32:64]
            cbb = cs.unsqueeze(1).to_broadcast([rows, H, 32])
            sbb = sn.unsqueeze(1).to_broadcast([rows, H, 32])
            t1, r1 = wf.next()
            t2, r2 = wf.next()
            a1 = t1[0:rows, 0:H * 32].rearrange("p (h d) -> p h d", h=H)
            a2 = t1[0:rows, 256:256 + H * 32].rearrange("p (h d) -> p h d", h=H)
            a3 = t2[0:rows, 0:H * 32].rearrange("p (h d) -> p h d", h=H)
            a4 = t2[0:rows, 256:256 + H * 32].rearrange("p (h d) -> p h d", h=H)
            V(lambda e: e.tensor_tensor(a1, x1, cbb, ALU.mult), rs, [r1])
            V(lambda e: e.tensor_tensor(a2, x2, sbb, ALU.mult), rs, [r1])
            V(lambda e: e.tensor_tensor(a3, x1, sbb, ALU.mult), rs, [r2])
            V(lambda e: e.tensor_tensor(a4, x2, cbb, ALU.mult), rs, [r2])
            V(lambda e: e.tensor_tensor(dst[:, :, 0:32], a1, a2, ALU.subtract), [r1], rd)
            V(lambda e: e.tensor_tensor(dst[:, :, 32:64], a3, a4, ALU.add), [r2], rd)

        def headnorm(src, H, g_bc, out, rs, ro, rows=128):
            sq, rq_ = wf.next()
            sqv = sq[0:rows, 0:H * 64].rearrange("p (h d) -> p h d", h=H)
            ss, rss = ws.next()
            SC(lambda e: e.activation(sqv, src, AF.Square), rs, [rq_])
            V(lambda e: e.tensor_reduce(ss[0:rows, 0:H], sqv, AX.X, ALU.add), [rq_], [rss])
            SC(lambda e: e.activation(ss[0:rows, 0:H], ss[0:rows, 0:H], AF.Sqrt, bias=EPS, scale=1.0 / 64), [rss], [rss])
            V(lambda e: e.reciprocal(ss[0:rows, 0:H], ss[0:rows, 0:H]), [rss], [rss])
            V(lambda e: e.tensor_tensor(out, src, ss[0:rows, 0:H].unsqueeze(2).to_broadcast([rows, H, 64]), ALU.mult), rs + [rss], ro)
            V(lambda e: e.tensor_tensor(out, out, g_bc, ALU.mult), ro + [r_small], ro)

        def norm_mod_tile(xt, rx, l, b, wh_g, wh_s, dstT, rdst, i):
            junk, rj = wbf.next()
            j2, rj2 = wbf.next()
            ss, rss = ws.next()
            SC(lambda e: e.activation(junk[:, 0:512], xt[:, 0:512], AF.Square, accum_out=ss[:, 0:1]), rx, [rj, rss])
            SC(lambda e: e.activation(junk[:, 0:512], xt[:, 512:1024], AF.Square, accum_out=ss[:, 1:2]), rx, [rj, rss])
            V(lambda e: e.tensor_tensor(ss[:, 0:1], ss[:, 0:1], ss[:, 1:2], ALU.add), [rss], [rss])
            SC(lambda e: e.activation(ss[:, 0:1], ss[:, 0:1], AF.Sqrt, bias=EPS, scale=1.0 / DM), [rss], [rss])
            V(lambda e: e.reciprocal(ss[:, 0:1], ss[:, 0:1]), [rss], [rss])
            V(lambda e: e.tensor_scalar(junk[:, 0:512], xt[:, 0:512], ss[:, 0:1], None, op0=ALU.mult), rx + [rss], [rj])
            V(lambda e: e.tensor_scalar(j2[:, 0:512], xt[:, 512:1024], ss[:, 0:1], None, op0=ALU.mult), rx + [rss], [rj2])
            pt, rp = pb.next()
            for k in range(8):
                srcb = junk if k < 4 else j2
                T(lambda e, k=k, srcb=srcb: e.transpose(pt[:, k, :], srcb[:, (k % 4) * 128:(k % 4 + 1) * 128], ident_b), [rj, rj2, r_c], [rp])
            tm1, rt1 = wf.next()
            tm2, rt2 = wf.next()
            gm = modT[:, l, wh_g, :, b]
            sh = modT[:, l, wh_s, :, b]
            for hh, tm, rt in ((0, tm1, rt1), (1, tm2, rt2)):
                tv = tm[:].rearrange("p (k t) -> p k t", k=4)
                V(lambda e, hh=hh, tv=tv: e.tensor_tensor(tv, pt[:, hh * 4:(hh + 1) * 4, :], gm[:, hh * 4:(hh + 1) * 4].unsqueeze(2).to_broadcast([128, 4, 128]), ALU.mult), [rp, r_mod], [rt])
                V(lambda e, hh=hh, tv=tv: e.tensor_tensor(dstT[:, hh * 4:(hh + 1) * 4, i * 128:(i + 1) * 128], tv, sh[:, hh * 4:(hh + 1) * 4].unsqueeze(2).to_broadcast([128, 4, 128]), ALU.add), [rt, r_mod], rdst)

        def transposes(src_list, rs, dst_fn, rows=128):
            pt, rp = pb.next()
            for n_, sap in enumerate(src_list):
                T(lambda e, n_=n_, sap=sap: e.transpose(pt[0:rows, n_, :], sap, ident_b), rs + [r_c], [rp])
            dst_fn(pt, rp)

        def layer(b, l, cosT, sinT, cosC, sinC, r_tab):
            m_layer = mark()
            x_src = x_d[b] if l == 0 else xscr_d
            x_dst = out_d[b] if l == nlayers - 1 else xscr_d
            hT = A([128, 8, S], BF16, "hT")
            r_hT = [Res() for _ in range(NT)]
            wblk = [A([128, 8, 512], BF16, f"wblk{i}") for i in range(2)]
            wblk_r = [Res(), Res()]
            wctr = [0]

            def load_w(src_ap, ncols):
                i_ = wctr[0] % 2
                wctr[0] += 1
                fw.dma("gpsimd", dwb[i_], wblk[i_][:, :, 0:ncols], src_ap, writes=[wblk_r[i_]])
                return wblk[i_], wblk_r[i_]

            def w_in_cols(c0, n):
                return W["w_in"][l, :, c0:c0 + n].rearrange("(k p) n -> p k n", p=128)

            def proj_tm(i, wb, wr, ncols, coff=0):
                pt, rp = pf.next()
                for k in range(8):
                    T(lambda e, k=k, pt=pt: e.matmul(pt[:, 0:ncols], hT[:, k, i * 128:(i + 1) * 128], wb[:, k, coff:coff + ncols], start=(k == 0), stop=(k == 7)),
                      [r_hT[i], wr], [rp])
                return pt, rp

            m_x = mark()
            xt_ring = [A([128, DM], F32, f"xt{i}") for i in range(2)]
            xt_r = [Res(), Res()]
            for i in range(NT):
                xt, rx = xt_ring[i % 2], xt_r[i % 2]
                fw.dma("sync", dx[i % 2], xt[:], x_src[i * 128:(i + 1) * 128, :], reads=[r_xd], writes=[rx])
                norm_mod_tile(xt, [rx], l, b, 1, 0, hT, [r_hT[i]], i)
            release(m_x)
            dump("hT", hT[:], r_hT)
            if stop_after == "norm1":
                raise _Stop()

            m_ret = mark()
            QTr = A([64, 4, S], BF16, "QTr")
            KTr = A([64, 4, S], BF16, "KTr")
            kdec = A([128, NT, 256], BF16, "kdec")
            vtm = A([128, NT, 256], BF16, "vtm")
            sgt = A([128, NT, 256], BF16, "sgt")
            r_ret = [Res() for _ in range(NT)]
            wb, wr = load_w(w_in_cols(O_RQ, 512), 512)
            nxt_ = proj_tm(0, wb, wr, 512)
            for i in range(NT):
                pt, rp = nxt_
                nxt_ = proj_tm(i + 1, wb, wr, 512) if i + 1 < NT else None
                qk, rqk = wbf.next()
                rope(pt[:].rearrange("p (h d) -> p h d", h=8), qk[:].rearrange("p (h d) -> p h d", h=8), 8, cosT[:, i, :], sinT[:, i, :], [rp, r_tab], [rqk])
                V(lambda e, qk=qk, i=i: e.tensor_tensor(kdec[:, i, :].rearrange("p (h d) -> p h d", h=4), qk[:, 256:512].rearrange("p (h d) -> p h d", h=4),
                                                        CF("deck").unsqueeze(2).to_broadcast([128, 4, 64]), ALU.mult), [rqk, r_c], [r_ret[i]])

                def dst(pt2, rp2, i=i):
                    V(lambda e: e.tensor_scalar(QTr[:, :, i * 128:(i + 1) * 128], pt2[0:64, 0:4, :], 0.125, None, op0=ALU.mult), [rp2], [r_ret[i]])
                    V(lambda e: e.tensor_copy(KTr[:, :, i * 128:(i + 1) * 128], pt2[0:64, 4:8, :]), [rp2], [r_ret[i]])
                transposes([qk[:, n_ * 64:(n_ + 1) * 64] for n_ in range(8)], [rqk], dst, rows=64)
            if stop_after == "t1x":
                pt, rp = pf.next()
                T(lambda e, pt=pt: e.matmul(pt[:, 0:128], ident_b, ident_b, start=True, stop=True), [r_c], [rp])
                dm, rdm = wf.next()
                V(lambda e, pt=pt, dm=dm: e.tensor_copy(dm[:, 0:128], pt[:, 0:128]), [rp], [rdm])
                SC(lambda e, dm=dm: e.activation(dm[:, 128:256], dm[:, 0:128], AF.Copy), [rdm], [rdm])
                raise _Stop()
            if stop_after == "t1":
                raise _Stop()
            wb, wr = load_w(w_in_cols(O_RV, 512), 512)
            nxt_ = proj_tm(0, wb, wr, 512)
            for i in range(NT):
                pt, rp = nxt_
                nxt_ = proj_tm(i + 1, wb, wr, 512) if i + 1 < NT else None
                V(lambda e, pt=pt, i=i: e.tensor_copy(vtm[:, i, :], pt[:, 0:256]), [rp], [r_ret[i]])
                SC(lambda e, pt=pt, i=i: e.activation(sgt[:, i, :], pt[:, 256:512], AF.Silu), [rp], [r_ret[i]])
            if stop_after == "t2":
                raise _Stop()
            wb, wr = load_w(w_in_cols(O_GU, 512), 512)
            nxt_ = proj_tm(0, wb, wr, 512)
            for i in range(NT):
                pt, rp = nxt_
                nxt_ = proj_tm(i + 1, wb, wr, 512) if i + 1 < NT else None
                gu, rgu = wf.next()
                ss, rss = ws.next()
                SC(lambda e, pt=pt, gu=gu: e.activation(gu[:, 0:256], pt[:, 0:256], AF.Gelu_apprx_tanh), [rp], [rgu])
                SC(lambda e, pt=pt, gu=gu, ss=ss: e.activation(gu[:, 256:512], pt[:, 256:512], AF.Gelu_apprx_tanh, accum_out=ss[:, 0:1]), [rp], [rgu, rss])
                V(lambda e, ss=ss: e.tensor_scalar(ss[:, 1:2], ss[:, 0:1], -1.0 / 256, None, op0=ALU.mult), [rss], [rss])
                sq, rsq = wf.next()
                SC(lambda e, gu=gu, sq=sq, ss=ss: e.activation(sq[:, 0:256], gu[:, 256:512], AF.Square, bias=ss[:, 1:2], accum_out=ss[:, 2:3]), [rgu, rss], [rsq, rss])
                SC(lambda e, ss=ss: e.activation(ss[:, 2:3], ss[:, 2:3], AF.Sqrt, bias=EPS, scale=1.0 / 256), [rss], [rss])
                V(lambda e, ss=ss: e.reciprocal(ss[:, 2:3], ss[:, 2:3]), [rss], [rss])
                V(lambda e, gu=gu, sq=sq, ss=ss: e.tensor_scalar(sq[:, 0:256], gu[:, 256:512], ss[:, 1:2], ss[:, 2:3], op0=ALU.add, op1=ALU.mult), [rgu, rss], [rsq])
                vn, rvn = wbf.next()
                V(lambda e, sq=sq, vn=vn: e.tensor_tensor(vn[:, 0:256], sq[:, 0:256], gln_bc[:, l, :], ALU.mult), [rsq, r_small], [rvn])
                ps, rps = pf.next()
                for g in range(4):
                    T(lambda e, g=g, ps=ps, vn=vn: e.matmul(ps[:, g * 64:(g + 1) * 64], WTg[:, l, g, :], vn[:, g * 64:(g + 1) * 64], start=True, stop=True), [rvn, r_wtg], [rps])
                V(lambda e, ps=ps, sq=sq: e.tensor_tensor(sq[:, 256:512].rearrange("p (g c) -> p g c", g=4), ps[:, 0:256].rearrange("p (g c) -> p g c", g=4),
                                                          bTg[:, l, :].unsqueeze(2).to_broadcast([128, 4, 64]), ALU.add), [rps, r_wtg], [rsq])
                yg, ryg = wbf.next()
                V(lambda e, sq=sq, gu=gu, yg=yg: e.tensor_tensor(yg[:, 0:256], sq[:, 256:512], gu[:, 0:256], ALU.mult), [rsq, rgu], [ryg])

                def dst(pt2, rp2, i=i):
                    V(lambda e: e.tensor_copy(yT[:, 2:4, i * 128:(i + 1) * 128], pt2[:, 0:2, :]), [rp2], [r_yT[i]])
                transposes([yg[:, 0:128], yg[:, 128:256]], [ryg], dst)
            dump("QTr", QTr[:], r_ret)
            dump("KTr", KTr[:], r_ret)
            dump("vtm", vtm[:], r_ret)
            dump("sgt", sgt[:], r_ret)
            dump("kdec", kdec[:], r_ret)
            if stop_after == "t3":
                raise _Stop()
            stf = A([64, 4, 64], F32, "stf")
            stb = A([64, 4, 64], BF16, "stb")
            r_st = Res()
            V(lambda e: e.memset(stf[:], 0.0), [], [r_st])
            V(lambda e: e.memset(stb[:], 0.0), [], [r_st])
            for i in range(NT):
                cs = slice(i * 128, (i + 1) * 128)
                pS, rpS = pf.next()
                for h in range(4):
                    T(lambda e, h=h, pS=pS: e.matmul(pS[:, h * 128:(h + 1) * 128], KTr[:, h, cs], QTr[:, h, cs], start=True, stop=True), [r_ret[i]], [rpS])
                PT, rPT = wbf.next()
                V(lambda e, pS=pS, PT=PT: e.tensor_tensor(PT[:], pS[:], CF("decT"), ALU.mult), [rpS, r_c], [rPT])
                if stop_after in ("r1", "r1a"):
                    raise _Stop()
                pO, rpO = pf.next()
                for h in range(4):
                    pr, hp = h // 2, (h % 2) * 64
                    T(lambda e, h=h, pO=pO, PT=PT: e.matmul(pO[:, h * 64:(h + 1) * 64], PT[:, h * 128:(h + 1) * 128], vtm[:, i, h * 64:(h + 1) * 64], start=True, stop=True), [rPT, r_ret[i]], [rpO])
                    T(lambda e, h=h, pO=pO: e.matmul(pO[:, 256 + h * 64:256 + (h + 1) * 64], QTr[:, h, cs], stb[:, h, :], start=True, stop=True), [r_ret[i], r_st], [rpO])
                o, ro = wf.next()
                V(lambda e, pO=pO, o=o: e.tensor_tensor(o[:, 256:512].rearrange("p (h d) -> p h d", h=4), pO[:, 256:512].rearrange("p (h d) -> p h d", h=4),
                                                        CF("decq").unsqueeze(2).to_broadcast([128, 4, 64]), ALU.mult), [rpO, r_c], [ro])
                V(lambda e, pO=pO, o=o: e.tensor_tensor(o[:, 0:256], o[:, 256:512], pO[:, 0:256], ALU.add), [rpO, ro], [ro])
                if stop_after == "r2":
                    raise _Stop()
                pK, rpK = pf.next()
                for h in range(4):
                    T(lambda e, h=h, pK=pK: e.matmul(pK[0:64, h * 64:(h + 1) * 64], kdec[:, i, h * 64:(h + 1) * 64], vtm[:, i, h * 64:(h + 1) * 64], start=True, stop=True), [r_ret[i]], [rpK])
                for h in range(4):
                    V(lambda e, h=h, pK=pK: e.scalar_tensor_tensor(stf[:, h, :], stf[:, h, :], _DCHUNK[h], pK[0:64, h * 64:(h + 1) * 64],
                                                                   op0=ALU.mult, op1=ALU.add), [rpK, r_st], [r_st])
                V(lambda e: e.tensor_copy(stb[:], stf[:]), [r_st], [r_st])
                if stop_after == "r3":
                    raise _Stop()
                on, ron = wf.next()
                headnorm(o[:, 0:256].rearrange("p (h d) -> p h d", h=4), 4, gret_bc[:, l, :].rearrange("p (h d) -> p h d", h=4), on[:, 0:256].rearrange("p (h d) -> p h d", h=4), [ro], [ron])
                yr, ryr = wbf.next()
                V(lambda e, on=on, yr=yr, i=i: e.tensor_tensor(yr[:, 0:256], on[:, 0:256], sgt[:, i, :], ALU.mult), [ron, r_ret[i]], [ryr])

                def dst(pt2, rp2, i=i):
                    V(lambda e: e.tensor_copy(yT[:, 0:2, i * 128:(i + 1) * 128], pt2[:, 0:2, :]), [rp2], [r_yT[i]])
                transposes([yr[:, 0:128], yr[:, 128:256]], [ryr], dst)
            if stop_after == "ret":
                raise _Stop()
            release(m_ret)
            m_nsa = mark()
            KsT = A([64, 2, S], BF16, "KsT")
            KwT = A([64, 2, S], BF16, "KwT")
            kcr = A([64, 2, S], BF16, "kcr")
            vcr = A([64, 2, S], BF16, "vcr")
            Vs = A([128, NT, 2, 66], BF16, "Vs")
            Vw = A([128, NT, 2, 66], BF16, "Vw")
            gat = A([128, NT, 24], F32, "gat")
            KcT = A([64, 2, 128], BF16, "KcT")
            Vca = A([128, 2, 98], BF16, "Vca")
            r_nsa = [Res() for _ in range(NT)]
            m_cmp = mark()
            w1 = [A([64, 32, 128], BF16, f"w1{i}") for i in range(2)]
            w2 = [A([128, 64], BF16, f"w2{i}") for i in range(2)]
            peT = A([64, 2, 32], BF16, "peT")
            bpe = A([128, 2], F32, "bpe")
            r_raw = [Res() for _ in range(4)]
            r_cw = Res()
            r_cmp = Res()
            for wi, (n1, n2, npe) in enumerate((("cmp_w1_k", "cmp_w2_k", "cmp_pe_k"), ("cmp_w1_v", "cmp_w2_v", "cmp_pe_v"))):
                fw.dma("gpsimd", dmisc, w1[wi][:], W[n1][l].rearrange("l d h -> d l h"), writes=[r_cw])
                fw.dma("gpsimd", dmisc, w2[wi][:], W[n2][l], writes=[r_cw])
                fw.dma("gpsimd", dmisc, peT[0:64, wi, :], W[npe][l].rearrange("l d -> d l"), writes=[r_cw])
            fw.group_end(dmisc, [r_cw])
            V(lambda e: e.memset(Vs[:, :, :, 64:66], 1.0), [], r_nsa)
            V(lambda e: e.memset(Vw[:, :, :, 64:66], 1.0), [], r_nsa)
            V(lambda e: e.memset(Vca[:, :, 64:98], 1.0), [], [r_cmp])
            for g in range(2):
                V(lambda e, g=g: e.tensor_copy(Vca[:, g, 65:97], CB("ovl")), [r_c], [r_cmp])
            wb, wr = load_w(w_in_cols(O_KC, 256), 256)
            for tb in range(4):
                for wi, dstb in enumerate((kcr, vcr)):
                    for g in range(2):
                        pt, rp = pf.next()
                        for k in range(8):
                            T(lambda e, k=k, pt=pt, wi=wi, g=g: e.matmul(pt[0:64, :], wb[:, k, wi * 128 + g * 64:wi * 128 + (g + 1) * 64], hT[:, k, tb * 512:(tb + 1) * 512], start=(k == 0), stop=(k == 7)),
                              r_hT[tb * 4:tb * 4 + 4] + [wr], [rp])
                        SC(lambda e, pt=pt, dstb=dstb, g=g: e.activation(dstb[:, g, tb * 512:(tb + 1) * 512], pt[0:64, :], AF.Copy), [rp], [r_raw[tb]])
            dump("kcr", kcr[:], r_raw)
            if stop_after == "c0":
                raise _Stop()
            pbias, rpb = pf.next()
            for wi in range(2):
                for l_ in range(32):
                    T(lambda e, wi=wi, l_=l_: e.matmul(pbias[:, wi:wi + 1], w1[wi][0:64, l_, :], peT[0:64, wi, l_:l_ + 1], start=(l_ == 0), stop=(l_ == 31)), [r_cw], [rpb])
            V(lambda e: e.tensor_copy(bpe[:], pbias[:, 0:2]), [rpb], [r_cmp])
            dump("bpe", bpe[:], [r_cmp])
            if stop_after == "c1":
                raise _Stop()
            for wi, raw in enumerate((kcr, vcr)):
                ph, rph = pf.next()
                for g in range(2):
                    for l_ in range(32):
                        T(lambda e, g=g, l_=l_, wi=wi, raw=raw, ph=ph: e.matmul(ph[:, g * 128:g * 128 + 127], w1[wi][:, l_, :], raw[:, g, l_:l_ + 16 * 126 + 1:16],
                                                                                 start=(l_ == 0), stop=(l_ == 31)), r_raw + [r_cw], [rph])
                hb, rhb = wbf.next()
                V(lambda e, hb=hb: e.memset(hb[:, 0:256].rearrange("p (g n) -> p g n", g=2)[:, :, 127:128], 0.0), [], [rhb])
                for g in range(2):
                    SC(lambda e, ph=ph, hb=hb, wi=wi, g=g: e.activation(hb[:, g * 128:g * 128 + 127], ph[:, g * 128:g * 128 + 127],
                                                                     AF.Gelu_apprx_tanh, bias=bpe[:, wi:wi + 1]), [rph, r_cmp], [rhb])
                pk, rpk = pf.next()
                for g in range(2):
                    T(lambda e, g=g, pk=pk, hb=hb, wi=wi: e.matmul(pk[:, g * 64:(g + 1) * 64], hb[:, g * 128:(g + 1) * 128], w2[wi][:], start=True, stop=True), [rhb, r_cw], [rpk])
                if stop_after == "c2":
                    raise _Stop()
                if wi == 0:
                    kn, rkn = wf.next()
                    headnorm(pk[:, 0:128].rearrange("p (h d) -> p h d", h=2), 2, gk_bc[:, l, 0, :].unsqueeze(1).to_broadcast([128, 2, 64]),
                             kn[:, 0:128].rearrange("p (h d) -> p h d", h=2), [rpk], [rkn])
                    kr, rkr = wbf.next()
                    rope(kn[:, 0:128].rearrange("p (h d) -> p h d", h=2), kr[:, 0:128].rearrange("p (h d) -> p h d", h=2), 2, cosC[:, :], sinC[:, :], [rkn, r_tab], [rkr])

                    def dstk(pt2, rp2):
                        V(lambda e: e.tensor_copy(KcT[:, :, :], pt2[0:64, 0:2, :]), [rp2], [r_cmp])
                    transposes([kr[:, 0:64], kr[:, 64:128]], [rkr], dstk, rows=64)
                else:
                    V(lambda e, pk=pk: e.tensor_copy(Vca[:, :, 0:64], pk[:, 0:128].rearrange("p (g d) -> p g d", g=2)), [rpk], [r_cmp])
            dump("KcT", KcT[:], [r_cmp])
            if stop_after == "cmp":
                raise _Stop()
            release(m_cmp)
            QTn = A([64, 2, 4, S], BF16, "QTn")
            dump("Vca", Vca[:], [r_cmp])
            wb, wr = load_w(w_in_cols(O_NQ, 512), 512)
            nxt_ = proj_tm(0, wb, wr, 512)
            for i in range(NT):
                pt, rp = nxt_
                nxt_ = proj_tm(i + 1, wb, wr, 512) if i + 1 < NT else None
                qn, rqn = wf.next()
                headnorm(pt[:].rearrange("p (h d) -> p h d", h=8), 8, gq_bc[:, l, :].unsqueeze(1).to_broadcast([128, 8, 64]), qn[:].rearrange("p (h d) -> p h d", h=8), [rp], [rqn])
                qr, rqr = wbf.next()
                rope(qn[:].rearrange("p (h d) -> p h d", h=8), qr[:].rearrange("p (h d) -> p h d", h=8), 8, cosT[:, i, :], sinT[:, i, :], [rqn, r_tab], [rqr])

                def dst(pt2, rp2, i=i):
                    for g in range(2):
                        V(lambda e, g=g: e.tensor_copy(QTn[:, g, :, i * 128:(i + 1) * 128], pt2[0:64, g * 4:(g + 1) * 4, :]), [rp2], [r_nsa[i]])
                transposes([qr[:, n_ * 64:(n_ + 1) * 64] for n_ in range(8)], [rqr], dst, rows=64)
            if stop_after == "p4":
                raise _Stop()
            wb, wr = load_w(w_in_cols(O_KS, 512), 512)
            nxt_ = proj_tm(0, wb, wr, 512)
            for i in range(NT):
                pt, rp = nxt_
                nxt_ = proj_tm(i + 1, wb, wr, 512) if i + 1 < NT else None
                kn, rkn = wf.next()
                kr, rkr = wbf.next()
                for which, c0 in ((1, 0), (2, 256)):
                    o0 = 0 if which == 1 else 128
                    headnorm(pt[:, c0:c0 + 128].rearrange("p (h d) -> p h d", h=2), 2, gk_bc[:, l, which, :].unsqueeze(1).to_broadcast([128, 2, 64]),
                             kn[:, o0:o0 + 128].rearrange("p (h d) -> p h d", h=2), [rp], [rkn])
                rope(kn[:, 0:256].rearrange("p (h d) -> p h d", h=4), kr[:, 0:256].rearrange("p (h d) -> p h d", h=4), 4, cosT[:, i, :], sinT[:, i, :], [rkn, r_tab], [rkr])
                V(lambda e, pt=pt, i=i: e.tensor_copy(Vs[:, i, :, 0:64], pt[:, 128:256].rearrange("p (g d) -> p g d", g=2)), [rp], [r_nsa[i]])
                V(lambda e, pt=pt, i=i: e.tensor_copy(Vw[:, i, :, 0:64], pt[:, 384:512].rearrange("p (g d) -> p g d", g=2)), [rp], [r_nsa[i]])

                def dst(pt2, rp2, i=i):
                    V(lambda e: e.tensor_copy(KsT[:, :, i * 128:(i + 1) * 128], pt2[0:64, 0:2, :]), [rp2], [r_nsa[i]])
                    V(lambda e: e.tensor_copy(KwT[:, :, i * 128:(i + 1) * 128], pt2[0:64, 2:4, :]), [rp2], [r_nsa[i]])
                transposes([kr[:, n_ * 64:(n_ + 1) * 64] for n_ in range(4)], [rkr], dst, rows=64)
            if stop_after == "p5":
                raise _Stop()
            wb, wr = load_w(w_in_cols(O_GL, 24), 24)
            nxt_ = proj_tm(0, wb, wr, 24)
            for i in range(NT):
                pt, rp = nxt_
                nxt_ = proj_tm(i + 1, wb, wr, 24) if i + 1 < NT else None
                SC(lambda e, pt=pt, i=i: e.activation(gat[:, i, :], pt[:, 0:24], AF.Sigmoid), [rp], [r_nsa[i]])
            dump("QTn", QTn[:], r_nsa)
            dump("KsT", KsT[:], r_nsa)
            dump("gat", gat[:], r_nsa)
            if stop_after == "nsaproj":
                raise _Stop()
            nselT = [A([128, 128], BF16, f"nselT{g}") for g in range(2)]
            r_nsel = [Res(), Res()]
            nsel_ring = Ring([A([128, 32], BF16, f"nsel{q_}") for q_ in range(2)])
            for i in range(NT):
                cs = slice(i * 128, (i + 1) * 128)
                for g in range(2):
                    QT4 = QTn[:, g, :, cs]
                    pC, rpC = paccA.next()
                    pOw, rpOw = paccA.next()
                    pOs, rpOs = paccA.next()
                    pC3 = pC[:, 0:388].rearrange("p (r c) -> p r c", r=4)
                    jlo = max(0, i - 4)
                    pairs = [("c", 0)] + [("w", j) for j in range(jlo, i + 1)] + [("s", i)] + [("s", j) for j in range(i)]
                    first = {"w": jlo, "s": i}
                    last = {"w": i, "s": (i - 1 if i > 0 else i)}
                    state = {}

                    def emit_nsel_transpose():
                        nsel, rns = state["nsel"]
                        pt2, rp2 = pb.next()
                        T(lambda e: e.transpose(pt2[0:32, 0, :], nsel[:, 0:32], ident_b), [rns, r_c], [rp2])
                        V(lambda e: e.tensor_copy(nselT[g][0:32, :], pt2[0:32, 0, :]), [rp2], [r_nsel[g]])

                    def emit_S(kind, j):
                        pS, rpS = pfA.next()
                        pS3 = pS[:].rearrange("p (r q) -> p r q", r=4)
                        if kind == "c":
                            T(lambda e: e.matmul(pS3, KcT[:, g, :], QT4, start=True, stop=False), [r_cmp, r_nsa[i]], [rpS])
                            T(lambda e: e.matmul(pS3, ident_b, CB("cmaskT")[:, cs].unsqueeze(1).to_broadcast([128, 4, 128]), start=False, stop=True), [r_c], [rpS])
                            return pS, rpS
                        KT_ = KsT if kind == "s" else KwT
                        mk = None
                        if j == i:
                            mk = (ident_b, CB("causT").unsqueeze(1).to_broadcast([128, 4, 128]), [r_c])
                        elif kind == "s":
                            if "nselT_done" not in state:
                                emit_nsel_transpose()
                                state["nselT_done"] = True
                            mk = (CB("Esel", 32)[:, j * 128:(j + 1) * 128], nselT[g][0:32, :].unsqueeze(1).to_broadcast([32, 4, 128]), [r_c, r_nsel[g]])
                        elif j == i - 4:
                            mk = (ident_b, CB("antiT").unsqueeze(1).to_broadcast([128, 4, 128]), [r_c])
                        T(lambda e: e.matmul(pS3, KT_[:, g, j * 128:(j + 1) * 128], QT4, start=True, stop=(mk is None)), [r_nsa[j], r_nsa[i]], [rpS])
                        if mk is not None:
                            T(lambda e: e.matmul(pS3, mk[0], mk[1], start=False, stop=True), mk[2], [rpS])
                        return pS, rpS

                    def emit_PV(kind, j, pS, rpS):
                        Pm, rPm = wbf.next()
                        SC(lambda e: e.activation(Pm[:], pS[:], AF.Exp), [rpS], [rPm])
                        if kind == "c":
                            for r in range(4):
                                T(lambda e: e.matmul(pC[:, r * 97:(r + 1) * 97], Pm[:, r * 128:(r + 1) * 128], Vca[:, g, 0:97], start=(r == 0), stop=(r == 3)), [rPm, r_cmp], [rpC])
                            return
                        pO, rpO, Vt_ = (pOs, rpOs, Vs) if kind == "s" else (pOw, rpOw, Vw)
                        for r in range(4):
                            T(lambda e: e.matmul(pO[:, r * 65:(r + 1) * 65], Pm[:, r * 128:(r + 1) * 128], Vt_[:, j, g, 0:65], start=(j == first[kind] and r == 0), stop=(j == last[kind] and r == 3)),
                              [rPm, r_nsa[j]], [rpO])

                    def emit_nsel_chain():
                        den, rden = ws.next()
                        state["den"] = (den, rden)
                        den3 = den[:, 0:12].rearrange("p (b r) -> p b r", b=3)
                        V(lambda e: e.tensor_scalar(den3[:, 0, :], pC3[:, :, 64], 1e-30, None, op0=ALU.max), [rpC], [rden])
                        rd0, rrd0 = ws.next()
                        V(lambda e: e.reciprocal(rd0[:, 0:4], den3[:, 0, :]), [rden], [rrd0])
                        sc, rsc = ws.next()
                        V(lambda e: e.tensor_scalar(sc[:, 0:32], pC3[:, 0, 65:97], rd0[:, 0:1], None, op0=ALU.mult), [rpC, rrd0], [rsc])
                        for r in range(1, 4):
                            V(lambda e: e.scalar_tensor_tensor(sc[:, 0:32], pC3[:, r, 65:97], rd0[:, r:r + 1], sc[:, 0:32], op0=ALU.mult, op1=ALU.add), [rpC, rrd0, rsc], [rsc])
                        V(lambda e: e.tensor_tensor(sc[:, 0:32], sc[:, 0:32], CF("a01")[:, i * 32:(i + 1) * 32], ALU.mult), [rsc, r_c], [rsc])
                        V(lambda e: e.tensor_tensor(sc[:, 0:32], sc[:, 0:32], CF("addc")[:, i * 32:(i + 1) * 32], ALU.add), [rsc, r_c], [rsc])
                        t8, rt8 = ws.next()
                        V(lambda e: e.max(out=t8[:, 0:8], in_=sc[:, 0:32]), [rsc], [rt8])
                        nsel, rns = nsel_ring.next()
                        V(lambda e: e.tensor_scalar(nsel[:, 0:32], sc[:, 0:32], t8[:, 7:8], 1.0, op0=ALU.is_ge, op1=ALU.subtract), [rsc, rt8], [rns])
                        state["nsel"] = (nsel, rns)

                    cur = emit_S(*pairs[0])
                    for kk, (kind, j) in enumerate(pairs):
                        nxt = emit_S(*pairs[kk + 1]) if kk + 1 < len(pairs) else None
                        emit_PV(kind, j, *cur)
                        if kind == "c":
                            emit_nsel_chain()
                        cur = nxt
                    den, rden = state["den"]
                    den3 = den[:, 0:12].rearrange("p (b r) -> p b r", b=3)
                    outs = [(pOs, rpOs), (pOw, rpOw)]
                    pO3s = [pC3] + [po[:, 0:260].rearrange("p (r c) -> p r c", r=4) for po, _ in outs]
                    rall = [rpC] + [rr_ for _, rr_ in outs]
                    for br in (1, 2):
                        V(lambda e, br=br, den3=den3: e.tensor_scalar(den3[:, br, :], pO3s[br][:, :, 64], 1e-30, None, op0=ALU.max), [rall[br]], [rden])
                    cf_, rcf = ws.next()
                    cf3 = cf_[:, 0:12].rearrange("p (b r) -> p b r", b=3)
                    V(lambda e, cf_=cf_, den=den: e.reciprocal(cf_[:, 0:12], den[:, 0:12]), [rden], [rcf])
                    V(lambda e, cf3=cf3: e.tensor_tensor(cf3, cf3, gat[:, i, g * 12:(g + 1) * 12].rearrange("p (r b) -> p b r", b=3), ALU.mult), [rcf, r_nsa[i]], [rcf])
                    oa, roa = wf.next()
                    ob, rob = wf.next()
                    oa3 = oa[:, 0:256].rearrange("p (r d) -> p r d", r=4)
                    ob3 = ob[:, 0:256].rearrange("p (r d) -> p r d", r=4)
                    V(lambda e, oa3=oa3, cf3=cf3: e.tensor_tensor(oa3, pO3s[0][:, :, 0:64], cf3[:, 0, :].unsqueeze(2).to_broadcast([128, 4, 64]), ALU.mult), [rall[0], rcf], [roa])
                    for br in (1, 2):
                        V(lambda e, br=br, ob3=ob3, cf3=cf3: e.tensor_tensor(ob3, pO3s[br][:, :, 0:64], cf3[:, br, :].unsqueeze(2).to_broadcast([128, 4, 64]), ALU.mult), [rall[br], rcf], [rob])
                        if br == 1:
                            V(lambda e, oa=oa, ob=ob: e.tensor_tensor(oa[:, 0:256], oa[:, 0:256], ob[:, 0:256], ALU.add), [roa, rob], [roa])
                    yn, ryn = wbf.next()
                    V(lambda e, oa=oa, ob=ob, yn=yn: e.tensor_tensor(yn[:, 0:256], oa[:, 0:256], ob[:, 0:256], ALU.add), [roa, rob], [ryn])

                    def dst(pt3, rp3, i=i, g=g):
                        V(lambda e: e.tensor_copy(yT[:, 4 + 2 * g:6 + 2 * g, i * 128:(i + 1) * 128], pt3[:, 0:2, :]), [rp3], [r_yT[i]])
                    transposes([yn[:, 0:128], yn[:, 128:256]], [ryn], dst)
            dump("yT", yT[:], r_yT)
            if stop_after == "nsa":
                raise _Stop()
            release(m_nsa)
            release(m_layer)
            m_b = mark()
            xres = A([128, NT, DM], F32, "xres")
            r_x = [Res() for _ in range(NT)]
            gbc = A([128, 1, DM], F32, "gbc")
            r_gbc = Res()
            gates = A([128, NT, 16], F32, "gates")
            r_gates = [Res() for _ in range(NT)]
            wg = [A([128, 8, 512], BF16, f"wg{i}") for i in range(2)]
            wu = [A([128, 8, 512], BF16, f"wu{i}") for i in range(2)]
            wd = [A([128, 4, 1024], BF16, f"wd{i}") for i in range(2)]
            r_we = [Res(), Res()]
            AT = [A([128, 4, 512], BF16, f"AT{i}") for i in range(2)]
            r_AT = [Res(), Res()]
            xt2 = [A([128, DM], F32, f"xtb{i}") for i in range(1)] * 2
            xt2_r = [Res()] * 2
            wout = wg[0], wg[1]
            def make_gbc(wh):
                vi = 0
                for half in range(2):
                    pt, rp = pf.next()
                    for kk in range(4):
                        k = half * 4 + kk
                        dg, rdg = wf.next()
                        V(lambda e, dg=dg, k=k, wh=wh: e.tensor_scalar(dg[:, 0:128], ident_f, modT[:, l, wh, k, b:b + 1], None, op0=ALU.mult), [r_c, r_mod], [rdg])
                        T(lambda e, dg=dg, pt=pt, kk=kk: e.matmul(pt[:, kk * 128:(kk + 1) * 128], CF("ones_f"), dg[:, 0:128], start=True, stop=True), [rdg, r_c], [rp])
                    V(lambda e, pt=pt, vi=vi, half=half: e.tensor_copy(gbc[:, vi, half * 512:(half + 1) * 512], pt[:]), [rp], [r_gbc])
            make_gbc(2)
            for half in range(2):
                fw.dma("gpsimd", dmoe[half], wg[half][:], W["w_out"][l, :, half * 512:(half + 1) * 512].rearrange("(k p) n -> p k n", p=128), writes=[r_we[half]])
            for i in range(NT):
                cs = slice(i * 128, (i + 1) * 128)
                xt, rxt = xt2[i % 2], xt2_r[i % 2]
                fw.dma("sync", dx[i % 2], xt[:], x_src[cs, :], reads=[r_xd], writes=[rxt])
                for half in range(2):
                    pt, rp = pf.next()
                    for k in range(8):
                        T(lambda e, k=k, pt=pt, half=half: e.matmul(pt[:], yT[:, k, cs], wg[half][:, k, :], start=(k == 0), stop=(k == 7)), [r_yT[i], r_we[half]], [rp])
                    tm, rtm = wf.next()
                    V(lambda e, pt=pt, tm=tm, half=half: e.tensor_tensor(tm[:], pt[:], gbc[:, 0, half * 512:(half + 1) * 512], ALU.mult), [rp, r_gbc], [rtm])
                    V(lambda e, tm=tm, xt=xt, half=half, i=i: e.tensor_tensor(xres[:, i, half * 512:(half + 1) * 512], tm[:], xt[:, half * 512:(half + 1) * 512], ALU.add), [rtm, rxt], [r_x[i]])
            dump("xmid", xres[:], r_x)
            if stop_after == "wout":
                raise _Stop()
            make_gbc(5)
            prl, rprl = pacc.next()
            for i in range(NT):
                cs = slice(i * 128, (i + 1) * 128)
                norm_mod_tile(xres[:, i, :], [r_x[i]], l, b, 4, 3, yT, [r_yT[i]], i)
                for k in range(8):
                    T(lambda e: e.matmul(prl[:, i * 16:(i + 1) * 16], yT[:, k, cs], rw_sb[:, k, :], start=(k == 0), stop=(k == 7)), [r_yT[i], r_rw], [rprl])
            tE, rE = wf.next()
            tS, rS = wf.next()
            tA, rA = wf.next()
            tB, rB = wf.next()
            tm, rm = wf.next()
            L3 = prl[:, 0:256].rearrange("p (t e) -> p t e", t=NT)
            E3 = tE[:, 0:256].rearrange("p (t e) -> p t e", t=NT)
            S3 = tS[:, 0:256].rearrange("p (t e) -> p t e", t=NT)
            S4 = tS[:, 0:256].rearrange("p (tg e) -> p tg e", e=4)
            A3 = tA[:, 0:256].rearrange("p (t e) -> p t e", t=NT)
            A4 = tA[:, 0:256].rearrange("p (tg e) -> p tg e", e=4)
            B3 = tB[:, 0:256].rearrange("p (t e) -> p t e", t=NT)
            mx = tm[:, 0:16]
            se = tm[:, 16:32]
            m1 = tm[:, 64:128]
            m2 = tm[:, 128:192]
            gmx = tm[:, 32:48]
            t1 = tm[:, 192:208]
            t2 = tm[:, 208:224]
            s2 = tm[:, 224:240]
            bc16 = lambda ap: ap.unsqueeze(2).to_broadcast([128, NT, 16])
            V(lambda e: e.tensor_reduce(mx, L3, AX.X, ALU.max), [rprl], [rm])
            V(lambda e: e.tensor_tensor(E3, L3, bc16(mx), ALU.subtract), [rprl, rm], [rE])
            SC(lambda e: e.activation(tE[:, 0:256], tE[:, 0:256], AF.Exp), [rE], [rE])
            V(lambda e: e.tensor_reduce(se, E3, AX.X, ALU.add), [rE], [rm])
            V(lambda e: e.reciprocal(se, se), [rm], [rm])
            V(lambda e: e.tensor_tensor(E3, E3, bc16(se), ALU.mult), [rE, rm], [rE])
            V(lambda e: e.tensor_tensor(S3, E3, rb_bc[:].unsqueeze(1).to_broadcast([128, NT, 16]), ALU.add), [rE, r_small], [rS])
            V(lambda e: e.tensor_reduce(m1, S4, AX.X, ALU.max), [rS], [rm])
            V(lambda e: e.tensor_tensor(A4, S4, m1.unsqueeze(2).to_broadcast([128, 64, 4]), ALU.is_equal), [rS, rm], [rA])
            V(lambda e: e.scalar_tensor_tensor(tA[:, 0:256], tA[:, 0:256], -1e9, tS[:, 0:256], op0=ALU.mult, op1=ALU.add), [rA, rS], [rA])
            V(lambda e: e.tensor_reduce(m2, A4, AX.X, ALU.max), [rA], [rm])
            V(lambda e: e.tensor_tensor(m1, m1, m2, ALU.add), [rm], [rm])
            V(lambda e: e.tensor_reduce(gmx, m1.rearrange("p (t g) -> p t g", g=4), AX.X, ALU.max), [rm], [rm])
            V(lambda e: e.tensor_tensor(m2.rearrange("p (t g) -> p t g", g=4), m1.rearrange("p (t g) -> p t g", g=4), gmx.unsqueeze(2).to_broadcast([128, NT, 4]), ALU.is_ge), [rm], [rm])
            V(lambda e: e.tensor_scalar(m2, m2, 1e9, -1e9, op0=ALU.mult, op1=ALU.add), [rm], [rm])
            V(lambda e: e.tensor_tensor(A4, S4, m2.unsqueeze(2).to_broadcast([128, 64, 4]), ALU.add), [rS, rm], [rA])
            V(lambda e: e.tensor_reduce(t1, A3, AX.X, ALU.max), [rA], [rm])
            V(lambda e: e.tensor_tensor(B3, A3, bc16(t1), ALU.is_equal), [rA, rm], [rB])
            V(lambda e: e.scalar_tensor_tensor(tB[:, 0:256], tB[:, 0:256], -1e9, tA[:, 0:256], op0=ALU.mult, op1=ALU.add), [rB, rA], [rB])
            V(lambda e: e.tensor_reduce(t2, B3, AX.X, ALU.max), [rB], [rm])
            V(lambda e: e.tensor_tensor(B3, A3, bc16(t2), ALU.is_ge), [rA, rm], [rB])
            V(lambda e: e.tensor_tensor(tB[:, 0:256], tB[:, 0:256], tE[:, 0:256], ALU.mult), [rB, rE], [rB])
            V(lambda e: e.tensor_reduce(s2, B3, AX.X, ALU.add), [rB], [rm])
            V(lambda e: e.reciprocal(s2, s2), [rm], [rm])
            V(lambda e: e.tensor_tensor(gates[:], B3, bc16(s2), ALU.mult), [rB, rm], r_gates)
            dump("gates", gates[:], r_gates)
            dump("h2T", yT[:], r_yT)
            if stop_after == "router":
                raise _Stop()
            for ex in range(NE if not dbg.get("_moe_experts") else dbg["_moe_experts"][0][0]):
                sl = ex % 2
                fw.dma("gpsimd", dmoe[sl], wg[sl][:], W["moe_w_gate"][l, ex].rearrange("(k p) n -> p k n", p=128), writes=[r_we[sl]])
                fw.dma("gpsimd", dmoe[sl], wu[sl][:], W["moe_w_up"][l, ex].rearrange("(k p) n -> p k n", p=128), writes=[r_we[sl]])
                fw.dma("gpsimd", dmoe[sl], wd[sl][:], W["moe_w_down"][l, ex].rearrange("(k p) n -> p k n", p=128), writes=[r_we[sl]])
                G(lambda e, sl=sl: e.tensor_tensor(wd[sl][:], wd[sl][:], gbc[:, 0, :].unsqueeze(1).to_broadcast([128, 4, DM]), ALU.mult), [r_we[sl], r_gbc], [r_we[sl]])
                def stage1(tb):
                    at, rat = AT[tb % 2], r_AT[tb % 2]
                    for dc in range(4):
                        pG, rpG = pf.next()
                        pU, rpU = pf.next()
                        for k in range(8):
                            T(lambda e: e.matmul(pG[:], wg[sl][:, k, dc * 128:(dc + 1) * 128], yT[:, k, tb * 512:(tb + 1) * 512], start=(k == 0), stop=(k == 7)),
                              r_yT[tb * 4:tb * 4 + 4] + [r_we[sl]], [rpG])
                        for k in range(8):
                            T(lambda e: e.matmul(pU[:], wu[sl][:, k, dc * 128:(dc + 1) * 128], yT[:, k, tb * 512:(tb + 1) * 512], start=(k == 0), stop=(k == 7)),
                              r_yT[tb * 4:tb * 4 + 4] + [r_we[sl]], [rpU])
                        sg, rsg = wf.next()
                        SC(lambda e: e.activation(sg[:], pG[:], AF.Silu), [rpG], [rsg])
                        V(lambda e: e.tensor_tensor(at[:, dc, :], sg[:], pU[:], ALU.mult), [rsg, rpU], [rat])

                def stage2(tb):
                    at, rat = AT[tb % 2], r_AT[tb % 2]
                    for tt in range(4):
                        i = tb * 4 + tt
                        for dh in range(2):
                            pO, rpO = pacc.next()
                            for dc in range(4):
                                T(lambda e: e.matmul(pO[:], at[:, dc, tt * 128:(tt + 1) * 128], wd[sl][:, dc, dh * 512:(dh + 1) * 512], start=(dc == 0), stop=(dc == 3)),
                                  [rat, r_we[sl]], [rpO])
                            V(lambda e: e.scalar_tensor_tensor(xres[:, i, dh * 512:(dh + 1) * 512], pO[:], gates[:, i, ex:ex + 1], xres[:, i, dh * 512:(dh + 1) * 512],
                                                               op0=ALU.mult, op1=ALU.add), [rpO, r_gates[i], r_x[i]], [r_x[i]])

                stage1(0)
                for tb in range(4):
                    if tb + 1 < 4:
                        stage1(tb + 1)
                    stage2(tb)
            is_last = (l == nlayers - 1)
            for i in range(NT):
                fw.dma("sync", dout, x_dst[i * 128:(i + 1) * 128, :], xres[:, i, :], reads=[r_x[i]], writes=([] if is_last else [r_xd]))
            release(m_b)

        r_xd = Res()
        for b in range(nseq):
            m_tab = mark()
            cosT = A([128, NT, 32], F32, "cos")
            sinT = A([128, NT, 32], F32, "sin")
            cosC = A([128, 32], F32, "cosC")
            sinC = A([128, 32], F32, "sinC")
            r_tab = Res()
            m_tmp = mark()
            posi = A([128, 128], I32, "posi")
            posf = A([128, 128], F32, "posf")
            post = A([128, NT + 1], F32, "post")
            r_pos = Res()
            dpos = fw.dma_sem(f"dpos{b}")
            fw.dma("sync", dpos, posi[0:NT, :], pos_d[b].rearrange("(i p) -> i p", p=128), writes=[r_pos])
            V(lambda e: e.tensor_copy(posf[0:NT, :], posi[0:NT, :]), [r_pos], [r_pos])
            pt, rp = pf.next()
            T(lambda e, pt=pt: e.transpose(pt[:, 0:NT], posf[0:NT, :], ident_f[0:NT, 0:NT]), [r_pos, r_c], [rp])
            V(lambda e, pt=pt: e.tensor_copy(post[:, 0:NT], pt[:, 0:NT]), [rp], [r_pos])
            posc = A([128, 1], I32, "posc")
            dpos2 = fw.dma_sem(f"dposc{b}")
            r_posc = Res()
            fw.dma("sync", dpos2, posc[0:127, :], pos_d[b, 31:2048:16].unsqueeze(1), writes=[r_posc])
            V(lambda e: e.memset(post[:, NT:NT + 1], 0.0), [r_pos], [r_pos])
            V(lambda e: e.tensor_copy(post[0:127, NT:NT + 1], posc[0:127, :]), [r_pos, r_posc], [r_pos])

            def make_tables(n_t, pos_ap, cos_out, sin_out):
                ang = A([128, n_t, 32], F32, "ang")
                kf = A([128, n_t, 32], F32, "kf")
                ki = A([128, n_t, 32], I32, "ki")
                rr = A([128, n_t, 32], F32, "rr")
                r_a = Res()
                V(lambda e: e.tensor_tensor(ang[:], CF("invf").unsqueeze(1).to_broadcast([128, n_t, 32]),
                                            pos_ap.unsqueeze(2).to_broadcast([128, n_t, 32]), ALU.mult), [r_pos, r_c], [r_a])
                V(lambda e: e.tensor_scalar(kf[:], ang[:], 1.0 / TWO_PI, None, op0=ALU.mult), [r_a], [r_a])
                V(lambda e: e.tensor_copy(ki[:], kf[:]), [r_a], [r_a])
                V(lambda e: e.tensor_copy(kf[:], ki[:]), [r_a], [r_a])
                V(lambda e: e.scalar_tensor_tensor(ang[:], kf[:], -TWO_PI, ang[:], op0=ALU.mult, op1=ALU.add), [r_a], [r_a])
                for shift, outp in ((0.0, sin_out), (PI / 2, cos_out)):
                    V(lambda e, shift=shift: e.tensor_scalar(rr[:], ang[:], shift, None, op0=ALU.add), [r_a], [r_a])
                    V(lambda e: e.tensor_scalar(kf[:], rr[:], PI, -TWO_PI, op0=ALU.is_gt, op1=ALU.mult), [r_a], [r_a])
                    V(lambda e: e.tensor_tensor(rr[:], rr[:], kf[:], ALU.add), [r_a], [r_a])
                    V(lambda e: e.tensor_scalar(rr[:], rr[:], PI, -PI, op0=ALU.min, op1=ALU.max), [r_a], [r_a])
                    SC(lambda e, outp=outp: e.activation(outp, rr[:], AF.Sin), [r_a], [r_tab])

            make_tables(NT, post[:, 0:NT], cosT[:], sinT[:])
            make_tables(1, post[:, NT:NT + 1], cosC[:].unsqueeze(1), sinC[:].unsqueeze(1))
            release(m_tmp)
            dump("cosT", cosT[:], [r_tab])
            dump("sinC", sinC[:], [r_tab])

            try:
                for l in range(nlayers):
                    layer(b, l, cosT, sinT, cosC, sinC, r_tab)
            except _Stop:
                break
            release(m_tab)

        fw.barrier()
        fw.build()
    print('SBUF high-water', ar['hi'])
    return nc


_NC_CACHE = {}


def kernel(**inputs):
    n_cores = 8
    nseq = 2
    if "nc" not in _NC_CACHE:
        _NC_CACHE["nc"] = build_program(nseq=nseq, nlayers=2)
    nc = _NC_CACHE["nc"]
    x = np.ascontiguousarray(np.asarray(inputs["x"], dtype=np.float32))
    c = np.ascontiguousarray(np.asarray(inputs["c"], dtype=np.float32))
    pos = np.ascontiguousarray(np.asarray(inputs["positions"]).astype(np.int32))
    shared = {n: np.ascontiguousarray(np.asarray(inputs[n], dtype=np.float32)) for n, _ in WEIGHT_SPECS}
    shared["cf"] = _CF
    shared["cb"] = _CB
    in_maps = []
    for k in range(n_cores):
        m = dict(shared)
        m["x"] = x[k * nseq:(k + 1) * nseq]
        m["c"] = c[k * nseq:(k + 1) * nseq]
        m["positions"] = pos[k * nseq:(k + 1) * nseq]
        in_maps.append(m)
    res = run_bass_kernel_spmd(nc, in_maps, core_ids=list(range(n_cores)))
    out = np.concatenate([np.asarray(r["out"], dtype=np.float32) for r in res.results], axis=0)
    return out
```

```python
import numpy as np
from contextlib import ExitStack
import ml_dtypes
import concourse.bass as bass
import concourse.mybir as mybir
from concourse.bass_utils import run_bass_kernel_spmd

F32 = mybir.dt.float32
BF16 = mybir.dt.bfloat16
I32 = mybir.dt.int32
AF = mybir.ActivationFunctionType
ALU = mybir.AluOpType
AX = mybir.AxisListType

S = 2048
DM = 1024
NT = 16
NE = 16
EPS = 1e-6
NEG = -30000.0
PI = float(np.pi)
TWO_PI = float(2 * np.pi)
N_IN = 2840
import os
SAME_ENGINE_SYNC = os.environ.get("K_SES", "1") == "1"
RAW_ONLY_SAME_ENGINE = os.environ.get("K_RAWONLY", "1") == "1"


class _Rec:
    def __init__(self):
        self.call = None

    def __getattr__(self, name):
        def f(*a, **k):
            assert self.call is None
            self.call = (name, a, k)
            return self
        return f


def _bind(fn):
    rec = _Rec()
    fn(rec)
    name, a, k = rec.call
    return lambda e: getattr(e, name)(*a, **k)


class Res:
    __slots__ = ("w", "r")

    def __init__(self):
        self.w = None
        self.r = {}


class FW:
    ENGS = ("tensor", "vector", "scalar", "gpsimd", "sync")

    def __init__(self, nc, stack):
        self.nc = nc
        self.stack = stack
        self.ops = {e: [] for e in self.ENGS}
        self.sem = {}
        self.cnt = {}
        for e in self.ENGS:
            self.sem[e] = stack.enter_context(nc.semaphore("s_" + e))
            self.cnt[e] = 0
        self.seen = {e: {} for e in self.ENGS}
        self.dsems = []

    def dma_sem(self, name):
        s = self.stack.enter_context(self.nc.semaphore(name))
        d = {"sem": s, "cnt": 0, "key": "d_" + name}
        self.dsems.append(d)
        return d

    def _deps(self, eng, reads, writes):
        deps = {}

        def add(t, raw):
            if t is None:
                return
            key, sem, val = t
            if key == eng and (eng == "tensor" or not SAME_ENGINE_SYNC or (RAW_ONLY_SAME_ENGINE and not raw)):
                return
            if deps.get(key, (None, -1))[1] < val:
                deps[key] = (sem, val)

        for r in reads:
            add(r.w, True)
        for w in writes:
            add(w.w, False)
            for t in w.r.values():
                add(t, False)
        out = []
        for key, (sem, val) in deps.items():
            if self.seen[eng].get(key, -1) >= val:
                continue
            self.seen[eng][key] = val
            out.append((sem, val))
        return out

    def _mark(self, tok, reads, writes):
        for r in reads:
            r.r[tok[0]] = tok
        for w in writes:
            w.w = tok
            w.r = {}

    def op(self, eng, fn, reads=(), writes=()):
        waits = self._deps(eng, reads, writes)
        self.cnt[eng] += 1
        tok = (eng, self.sem[eng], self.cnt[eng])
        self._mark(tok, reads, writes)
        self.ops[eng].append((waits, _bind(fn), self.sem[eng], 1))
        return tok

    def dma(self, eng, dsem, out, in_, reads=(), writes=()):
        waits = self._deps(eng, reads, writes)
        dsem["cnt"] += 16
        tok = (dsem["key"], dsem["sem"], dsem["cnt"])
        self._mark(tok, reads, writes)
        self.ops[eng].append((waits, lambda e: e.dma_start(out=out, in_=in_), dsem["sem"], 16))
        return tok

    def group_end(self, dsem, resources):
        tok = (dsem["key"], dsem["sem"], dsem["cnt"])
        for r in resources:
            r.w = tok

    def barrier(self):
        toks = [(e, self.sem[e], self.cnt[e]) for e in self.ENGS if self.cnt[e] > 0]
        toks += [(d["key"], d["sem"], d["cnt"]) for d in self.dsems if d["cnt"] > 0]
        for eng in self.ENGS:
            waits = []
            for key, sem, val in toks:
                if key == eng:
                    continue
                if self.seen[eng].get(key, -1) >= val:
                    continue
                self.seen[eng][key] = val
                waits.append((sem, val))
            if waits:
                self.ops[eng].append((waits, None, None, 0))

    def build(self):
        nc = self.nc
        with nc.Block() as block:
            def mk(name):
                def body(e):
                    for waits, fn, sem, inc in self.ops[name]:
                        for (s, v) in waits:
                            e.wait_ge(s, v)
                        if fn is not None:
                            fn(e).then_inc(sem, inc)
                return body
            block.tensor(mk("tensor"))
            block.vector(mk("vector"))
            block.scalar(mk("scalar"))
            block.gpsimd(mk("gpsimd"))
            block.sync(mk("sync"))


class _Stop(Exception):
    pass


class Ring:
    def __init__(self, bufs):
        self.bufs = [(b, Res()) for b in bufs]
        self.i = 0

    def next(self):
        b = self.bufs[self.i % len(self.bufs)]
        self.i += 1
        return b


def _consts():
    f32 = np.float32
    c = {}
    c["ident_f"] = np.eye(128, dtype=f32)
    half = 32
    inv_freq = np.power(f32(10000.0), -np.arange(half, dtype=f32) / f32(half)).astype(f32)
    c["invf"] = np.tile(inv_freq[None, :], (128, 1)).astype(f32)
    t = np.arange(128)
    c["tril"] = (t[None, :] <= t[:, None]).astype(f32)
    H = 4
    log_gamma = np.log1p(-np.power(2.0, -5.0 - np.arange(H))).astype(np.float64)
    diff = t[None, :] - t[:, None]
    decT = np.zeros((128, H, 128), dtype=f32)
    for h in range(H):
        decT[:, h, :] = np.where(diff >= 0, np.exp(np.maximum(diff, 0) * log_gamma[h]), 0.0)
    c["decT"] = decT.reshape(128, H * 128)
    c["decq"] = np.exp((t[:, None] + 1.0) * log_gamma[None, :]).astype(f32)
    c["deck"] = np.exp((127.0 - t[:, None]) * log_gamma[None, :]).astype(f32)
    dchunk = [float(np.exp(128.0 * log_gamma[h])) for h in range(H)]
    tt = np.arange(S)
    j = np.arange(32)
    cur = tt // 64
    forced = (j[None, :] == 0) | (j[None, :] == cur[:, None]) | (j[None, :] == cur[:, None] - 1)
    allowed = (j[None, :] * 64) <= tt[:, None]
    a01 = (allowed & ~forced).astype(f32)
    addc = np.where(forced, 1e4, np.where(allowed, 0.0, -1.0)).astype(f32)
    c["a01"] = a01.reshape(NT, 128, 32).transpose(1, 0, 2).reshape(128, NT * 32)
    c["addc"] = addc.reshape(NT, 128, 32).transpose(1, 0, 2).reshape(128, NT * 32)
    ones = np.ones((128, 128), dtype=f32)
    c["ones_f"] = ones
    cf_names = ["ident_f", "invf", "tril", "decT", "decq", "deck", "a01", "addc", "ones_f"]
    cf_off = {}
    o = 0
    for n in cf_names:
        cf_off[n] = (o, c[n].shape[1])
        o += c[n].shape[1]
    cf = np.concatenate([c[n] for n in cf_names], axis=1).astype(f32)

    b = {}
    b["ident_b"] = np.eye(128, dtype=f32)
    key = t[:, None]
    q = t[None, :]
    b["causT"] = np.where(key <= q, 0.0, NEG).astype(f32)
    b["antiT"] = np.where(key > q, 0.0, NEG).astype(f32)
    n = np.arange(128)
    cm = np.where((16 * n[:, None] + 31) <= tt[None, :], 0.0, NEG).astype(f32)
    cm[127, :] = NEG
    b["cmaskT"] = cm
    E = np.zeros((128, NT, 128), dtype=f32)
    for jj in range(NT):
        for kk in range(128):
            E[2 * jj + kk // 64, jj, kk] = -NEG
    b["Esel"] = E.reshape(128, NT * 128)
    ov = np.zeros((128, 32), dtype=f32)
    for nn in range(127):
        for jj in range(32):
            if (16 * nn < 64 * jj + 64) and (16 * nn + 32 > 64 * jj):
                ov[nn, jj] = 1.0
    b["ovl"] = ov
    b["ones_b"] = np.ones((128, 128), dtype=f32)
    cb_names = ["ident_b", "causT", "antiT", "cmaskT", "Esel", "ovl", "ones_b"]
    cb_off = {}
    o = 0
    for nme in cb_names:
        cb_off[nme] = (o, b[nme].shape[1])
        o += b[nme].shape[1]
    cb = np.concatenate([b[nme] for nme in cb_names], axis=1).astype(ml_dtypes.bfloat16)
    return cf, cf_off, cb, cb_off, dchunk


_CF, _CF_OFF, _CB, _CB_OFF, _DCHUNK = _consts()

O_RQ, O_RK, O_RV, O_RG, O_GU, O_GV, O_NQ = 0, 256, 512, 768, 1024, 1280, 1536
O_KC, O_VC, O_KS, O_VS, O_KW, O_VW, O_GL = 2048, 2176, 2304, 2432, 2560, 2688, 2816

WEIGHT_SPECS = [
    ("ada_w", [2, 1024, 6144]), ("ada_b", [2, 6144]), ("norm_mix_g", [2, 1024]),
    ("norm_ffn_g", [2, 1024]), ("w_in", [2, 1024, N_IN]), ("w_out", [2, 1024, 1024]),
    ("ret_norm_g", [2, 4, 64]), ("gmlp_ln_g", [2, 256]), ("gmlp_ws", [2, 4, 128, 128]),
    ("gmlp_b", [2, 4, 128]), ("nsa_q_norm_g", [2, 64]), ("nsa_k_norm_g", [2, 3, 64]),
    ("cmp_pe_k", [2, 32, 64]), ("cmp_pe_v", [2, 32, 64]), ("cmp_w1_k", [2, 32, 64, 128]),
    ("cmp_w2_k", [2, 128, 64]), ("cmp_w1_v", [2, 32, 64, 128]), ("cmp_w2_v", [2, 128, 64]),
    ("router_w", [1024, 16]), ("router_b", [16]), ("moe_w_gate", [2, 16, 1024, 512]),
    ("moe_w_up", [2, 16, 1024, 512]), ("moe_w_down", [2, 16, 512, 1024]),
]


def build_program(nseq=2, nlayers=2, dbg=None, stop_after=None):
    dbg = dbg or {}
    nc = bass.Bass("TRN2", target_bir_lowering=False)

    def D(name, shape, dt, kind="ExternalInput"):
        return nc.dram_tensor(name, shape, dt, kind=kind).ap()

    x_d = D("x", [nseq, S, DM], F32)
    c_d = D("c", [nseq, DM], F32)
    pos_d = D("positions", [nseq, S], I32)
    W = {n: D(n, shp, F32) for n, shp in WEIGHT_SPECS}
    cf_d = D("cf", list(_CF.shape), F32)
    cb_d = D("cb", list(_CB.shape), BF16)
    out_d = D("out", [nseq, S, DM], F32, kind="ExternalOutput")
    xscr_d = D("xscr", [S, DM], F32, kind="Internal")
    dbg_d = {k: D("dbg_" + k, shp, dt, kind="ExternalOutput") for k, (shp, dt) in dbg.items() if not k.startswith("_")}

    with ExitStack() as st:
        fw = FW(nc, st)
        st.enter_context(nc.allow_non_contiguous_dma(reason="small strided parameter loads"))

        ar = {"cur": 16512, "hi": 0}
        cnt = [0]

        def A(shape, dt, name=None):
            nb = int(np.prod(shape[1:])) * (4 if dt in (F32, I32) else 2)
            off = (ar["cur"] + 63) // 64 * 64
            ar["cur"] = off + nb
            ar["hi"] = max(ar["hi"], ar["cur"])
            assert ar["cur"] <= 229376, f"SBUF overflow {ar['cur']}"
            cnt[0] += 1
            return nc.alloc_sbuf_tensor_at(f"{name or 't'}_{cnt[0]}", list(shape), dt, offset=off)

        def mark():
            return ar["cur"]

        def release(m):
            fw.barrier()
            ar["cur"] = m

        pf = Ring([nc.alloc_psum_tensor(f"pf{i}", [128, 512], F32) for i in range(3)])
        pacc = Ring([nc.alloc_psum_tensor(f"pa{i}", [128, 512], F32) for i in range(3)])
        pfA = Ring([])
        pfA.bufs = pf.bufs[0:2]
        paccA = Ring([])
        paccA.bufs = [pf.bufs[2]] + pacc.bufs
        pb = Ring([nc.alloc_psum_tensor(f"pb{i}", [128, 8, 128], BF16) for i in range(2)])

        def V(fn, r=(), w=()):
            return fw.op("vector", fn, r, w)

        def SC(fn, r=(), w=()):
            return fw.op("scalar", fn, r, w)

        def T(fn, r=(), w=()):
            return fw.op("tensor", fn, r, w)

        def G(fn, r=(), w=()):
            return fw.op("gpsimd", fn, r, w)

        cf = A(list(_CF.shape), F32, "cf")
        cb = A(list(_CB.shape), BF16, "cb")
        r_c = Res()
        dconst = fw.dma_sem("dconst")
        fw.dma("sync", dconst, cf[:], cf_d, writes=[r_c])
        fw.dma("sync", dconst, cb[:], cb_d, writes=[r_c])

        def CF(name, rows=128):
            o, n = _CF_OFF[name]
            return cf[0:rows, o:o + n]

        def CB(name, rows=128):
            o, n = _CB_OFF[name]
            return cb[0:rows, o:o + n]

        ident_f = CF("ident_f")
        ident_b = CB("ident_b")

        modT = A([128, 2, 6, 8, nseq], F32, "modT")
        r_mod = Res()
        gmixT = A([128, 2, 2, 8], F32, "gT")
        r_g = Res()
        WTg = A([128, 2, 4, 128], BF16, "WTg")
        bTg = A([128, 2, 4], F32, "bTg")
        r_wtg = Res()
        gq_bc = A([128, 2, 64], F32, "gq")
        gk_bc = A([128, 2, 3, 64], F32, "gk")
        gret_bc = A([128, 2, 256], F32, "gret")
        gln_bc = A([128, 2, 256], F32, "gln")
        rb_bc = A([128, 16], F32, "rb")
        rw_sb = A([128, 8, 16], BF16, "rw")
        r_small = Res()
        dsm = fw.dma_sem("dsmall")
        for l in range(nlayers):
            fw.dma("sync", dsm, gq_bc[:, l, :], W["nsa_q_norm_g"][l].partition_broadcast(128), writes=[r_small])
            fw.dma("sync", dsm, gk_bc[:, l, :, :], W["nsa_k_norm_g"][l].partition_broadcast(128), writes=[r_small])
            fw.dma("sync", dsm, gret_bc[:, l, :], W["ret_norm_g"][l].rearrange("h d -> (h d)").partition_broadcast(128), writes=[r_small])
            fw.dma("sync", dsm, gln_bc[:, l, :], W["gmlp_ln_g"][l].partition_broadcast(128), writes=[r_small])
        fw.dma("sync", dsm, rb_bc[:], W["router_b"].partition_broadcast(128), writes=[r_small])
        drw = fw.dma_sem("drw")
        r_rw = Res()
        fw.dma("gpsimd", drw, rw_sb[:], W["router_w"].rearrange("(k p) e -> p k e", p=128), writes=[r_rw])
        fw.group_end(dconst, [r_c])
        fw.group_end(dsm, [r_small])
        V(lambda e: e.tensor_scalar(gq_bc[:], gq_bc[:], 0.125, None, op0=ALU.mult), [r_small], [r_small])

        m_setup = mark()
        stg = A([128, 512], F32, "stg")
        r_stg = Res()
        dstg = fw.dma_sem("dstg")

        def load_T(src_ap, rows, dst_fn, post=None):
            fw.dma("sync", dstg, stg[0:rows, 0:128], src_ap, writes=[r_stg])
            if post is not None:
                post(stg[0:rows, 0:128])
            pt, rp = pf.next()
            T(lambda e: e.transpose(pt[:, 0:rows], stg[0:rows, 0:128], ident_f[0:rows, 0:rows]), [r_stg, r_c], [rp])
            dst_fn(pt[:, 0:rows], rp)

        scT = A([128, 8, nseq], BF16, "scT")
        r_scT = Res()

        def post_silu(ap):
            SC(lambda e: e.activation(ap, ap, AF.Silu), [r_stg], [r_stg])

        def dst_sc(ps, rp):
            V(lambda e: e.tensor_copy(scT[:].rearrange("p k b -> p b k"), ps.rearrange("p (b k) -> p b k", k=8)), [rp], [r_scT])

        load_T(c_d.rearrange("b (k p) -> (b k) p", p=128), 8 * nseq, dst_sc, post_silu)

        adab = A([128, 48], F32, "adab")
        r_adab = Res()
        adaw = [A([128, 8, 768], BF16, f"adaw{i}") for i in range(2)]
        adaw_r = [Res(), Res()]
        adaw_d = [fw.dma_sem("dadaw0"), fw.dma_sem("dadaw1")]
        blk = 0
        for l in range(nlayers):
            def dst_b(ps, rp):
                V(lambda e: e.tensor_copy(adab[:], ps), [rp], [r_adab])
            load_T(W["ada_b"][l].rearrange("(j p) -> j p", p=128), 48, dst_b)
            for gi, gname in enumerate(["norm_mix_g", "norm_ffn_g"]):
                def dst_g(ps, rp, gi=gi, l=l):
                    V(lambda e: e.tensor_copy(gmixT[:, l, gi, :], ps), [rp], [r_g])
                load_T(W[gname][l].rearrange("(k p) -> k p", p=128), 8, dst_g)
            for jb in range(8):
                wb, wr, wd = adaw[blk % 2], adaw_r[blk % 2], adaw_d[blk % 2]
                blk += 1
                fw.dma("gpsimd", wd, wb[:], W["ada_w"][l, :, jb * 768:(jb + 1) * 768].rearrange("(k p) n -> p k n", p=128), writes=[wr])
                pt, rp = pf.next()
                for jj in range(6):
                    for k in range(8):
                        T(lambda e, jj=jj, k=k, wb=wb, pt=pt: e.matmul(pt[:, jj * nseq:(jj + 1) * nseq], wb[:, k, jj * 128:(jj + 1) * 128], scT[:, k, :], start=(k == 0), stop=(k == 7)),
                          [wr, r_scT], [rp])
                for jj in range(6):
                    j = jb * 6 + jj
                    V(lambda e, jj=jj, j=j, pt=pt, l=l: e.tensor_tensor(modT[:, l, j // 8, j % 8, :], pt[:, jj * nseq:(jj + 1) * nseq],
                                                               adab[:, j:j + 1].to_broadcast([128, nseq]), ALU.add), [rp, r_adab], [r_mod])
        for l in range(nlayers):
            for gi, wh in ((0, 1), (1, 4)):
                V(lambda e, l=l, gi=gi, wh=wh: e.scalar_tensor_tensor(modT[:, l, wh, :, :], modT[:, l, wh, :, :], 1.0,
                                                                       gmixT[:, l, gi, :].unsqueeze(2).to_broadcast([128, 8, nseq]),
                                                                       op0=ALU.add, op1=ALU.mult), [r_mod, r_g], [r_mod])

        for l in range(nlayers):
            wst = A([128, 4, 128], F32, "wst")
            r_wst = Res()
            dwst = fw.dma_sem(f"dwst{l}")
            fw.dma("sync", dwst, wst[:], W["gmlp_ws"][l].rearrange("g t s -> t g s"), writes=[r_wst])
            V(lambda e, wst=wst: e.tensor_tensor(wst[:], wst[:], CF("tril").unsqueeze(1).to_broadcast([128, 4, 128]), ALU.mult), [r_wst, r_c], [r_wst])
            pt, rp = pf.next()
            for g in range(4):
                T(lambda e, g=g, pt=pt, wst=wst: e.transpose(pt[:, g * 128:(g + 1) * 128], wst[:, g, :], ident_f), [r_wst, r_c], [rp])
            V(lambda e, pt=pt, l=l: e.tensor_copy(WTg[:, l, :, :], pt[:].rearrange("p (g t) -> p g t", g=4)), [rp], [r_wtg])

            def dst_bt(ps, rp, l=l):
                V(lambda e: e.tensor_copy(bTg[:, l, :], ps), [rp], [r_wtg])
            load_T(W["gmlp_b"][l], 4, dst_bt)
        release(m_setup)

        dbg_sem = fw.dma_sem("ddbg")
        out_toks = []

        def dump(name, ap, res_list):
            if name in dbg_d:
                t = fw.dma("sync", dbg_sem, dbg_d[name], ap, reads=res_list)
                rr = Res()
                rr.w = t
                out_toks.append(rr)

        dump("modT", modT[:], [r_mod])
        dump("WTg", WTg[:], [r_wtg])

        yT = A([128, 8, S], BF16, "yT")
        r_yT = [Res() for _ in range(NT)]
        wf = Ring([A([128, 512], F32, "wf") for _ in range(6)])
        wbf = Ring([A([128, 512], BF16, "wbf") for _ in range(3)])
        ws = Ring([A([128, 32], F32, "ws") for _ in range(10)])
        dx = [fw.dma_sem(f"dx{i}") for i in range(2)]
        dwb = [fw.dma_sem(f"dwb{i}") for i in range(2)]
        dmoe = [fw.dma_sem(f"dmoe{i}") for i in range(2)]
        dmisc = fw.dma_sem("dmisc")
        dout = fw.dma_sem("dout")
        m_seq = mark()

        def rope(src, dst, H, cs, sn, rs, rd, rows=128):
            x1 = src[:, :, 0:32]
            x2 = src[:, :, 32:64]
            cbb = cs.unsqueeze(1).to_broadcast([rows, H, 32])
            sbb = sn.unsqueeze(1).to_broadcast([rows, H, 32])
            t1, r1 = wf.next()
            t2, r2 = wf.next()
            a1 = t1[0:rows, 0:H * 32].rearrange("p (h d) -> p h d", h=H)
            a2 = t1[0:rows, 256:256 + H * 32].rearrange("p (h d) -> p h d", h=H)
            a3 = t2[0:rows, 0:H * 32].rearrange("p (h d) -> p h d", h=H)
            a4 = t2[0:rows, 256:256 + H * 32].rearrange("p (h d) -> p h d", h=H)
            V(lambda e: e.tensor_tensor(a1, x1, cbb, ALU.mult), rs, [r1])
            V(lambda e: e.tensor_tensor(a2, x2, sbb, ALU.mult), rs, [r1])
            V(lambda e: e.tensor_tensor(a3, x1, sbb, ALU.mult), rs, [r2])
            V(lambda e: e.tensor_tensor(a4, x2, cbb, ALU.mult), rs, [r2])
            V(lambda e: e.tensor_tensor(dst[:, :, 0:32], a1, a2, ALU.subtract), [r1], rd)
            V(lambda e: e.tensor_tensor(dst[:, :, 32:64], a3, a4, ALU.add), [r2], rd)

        def headnorm(src, H, g_bc, out, rs, ro, rows=128):
            sq, rq_ = wf.next()
            sqv = sq[0:rows, 0:H * 64].rearrange("p (h d) -> p h d", h=H)
            ss, rss = ws.next()
            SC(lambda e: e.activation(sqv, src, AF.Square), rs, [rq_])
            V(lambda e: e.tensor_reduce(ss[0:rows, 0:H], sqv, AX.X, ALU.add), [rq_], [rss])
            SC(lambda e: e.activation(ss[0:rows, 0:H], ss[0:rows, 0:H], AF.Sqrt, bias=EPS, scale=1.0 / 64), [rss], [rss])
            V(lambda e: e.reciprocal(ss[0:rows, 0:H], ss[0:rows, 0:H]), [rss], [rss])
            V(lambda e: e.tensor_tensor(out, src, ss[0:rows, 0:H].unsqueeze(2).to_broadcast([rows, H, 64]), ALU.mult), rs + [rss], ro)
            V(lambda e: e.tensor_tensor(out, out, g_bc, ALU.mult), ro + [r_small], ro)

        def norm_mod_tile(xt, rx, l, b, wh_g, wh_s, dstT, rdst, i):
            junk, rj = wbf.next()
            j2, rj2 = wbf.next()
            ss, rss = ws.next()
            SC(lambda e: e.activation(junk[:, 0:512], xt[:, 0:512], AF.Square, accum_out=ss[:, 0:1]), rx, [rj, rss])
            SC(lambda e: e.activation(junk[:, 0:512], xt[:, 512:1024], AF.Square, accum_out=ss[:, 1:2]), rx, [rj, rss])
            V(lambda e: e.tensor_tensor(ss[:, 0:1], ss[:, 0:1], ss[:, 1:2], ALU.add), [rss], [rss])
            SC(lambda e: e.activation(ss[:, 0:1], ss[:, 0:1], AF.Sqrt, bias=EPS, scale=1.0 / DM), [rss], [rss])
            V(lambda e: e.reciprocal(ss[:, 0:1], ss[:, 0:1]), [rss], [rss])
            V(lambda e: e.tensor_scalar(junk[:, 0:512], xt[:, 0:512], ss[:, 0:1], None, op0=ALU.mult), rx + [rss], [rj])
            V(lambda e: e.tensor_scalar(j2[:, 0:512], xt[:, 512:1024], ss[:, 0:1], None, op0=ALU.mult), rx + [rss], [rj2])
            pt, rp = pb.next()
            for k in range(8):
                srcb = junk if k < 4 else j2
                T(lambda e, k=k, srcb=srcb: e.transpose(pt[:, k, :], srcb[:, (k % 4) * 128:(k % 4 + 1) * 128], ident_b), [rj, rj2, r_c], [rp])
            tm1, rt1 = wf.next()
            tm2, rt2 = wf.next()
            gm = modT[:, l, wh_g, :, b]
            sh = modT[:, l, wh_s, :, b]
            for hh, tm, rt in ((0, tm1, rt1), (1, tm2, rt2)):
                tv = tm[:].rearrange("p (k t) -> p k t", k=4)
                V(lambda e, hh=hh, tv=tv: e.tensor_tensor(tv, pt[:, hh * 4:(hh + 1) * 4, :], gm[:, hh * 4:(hh + 1) * 4].unsqueeze(2).to_broadcast([128, 4, 128]), ALU.mult), [rp, r_mod], [rt])
                V(lambda e, hh=hh, tv=tv: e.tensor_tensor(dstT[:, hh * 4:(hh + 1) * 4, i * 128:(i + 1) * 128], tv, sh[:, hh * 4:(hh + 1) * 4].unsqueeze(2).to_broadcast([128, 4, 128]), ALU.add), [rt, r_mod], rdst)

        def transposes(src_list, rs, dst_fn, rows=128):
            pt, rp = pb.next()
            for n_, sap in enumerate(src_list):
                T(lambda e, n_=n_, sap=sap: e.transpose(pt[0:rows, n_, :], sap, ident_b), rs + [r_c], [rp])
            dst_fn(pt, rp)

        def layer(b, l, cosT, sinT, cosC, sinC, r_tab):
            m_layer = mark()
            x_src = x_d[b] if l == 0 else xscr_d
            x_dst = out_d[b] if l == nlayers - 1 else xscr_d
            hT = A([128, 8, S], BF16, "hT")
            r_hT = [Res() for _ in range(NT)]
            wblk = [A([128, 8, 512], BF16, f"wblk{i}") for i in range(2)]
            wblk_r = [Res(), Res()]
            wctr = [0]

            def load_w(src_ap, ncols):
                i_ = wctr[0] % 2
                wctr[0] += 1
                fw.dma("gpsimd", dwb[i_], wblk[i_][:, :, 0:ncols], src_ap, writes=[wblk_r[i_]])
                return wblk[i_], wblk_r[i_]

            def w_in_cols(c0, n):
                return W["w_in"][l, :, c0:c0 + n].rearrange("(k p) n -> p k n", p=128)

            def proj_tm(i, wb, wr, ncols, coff=0):
                pt, rp = pf.next()
                for k in range(8):
                    T(lambda e, k=k, pt=pt: e.matmul(pt[:, 0:ncols], hT[:, k, i * 128:(i + 1) * 128], wb[:, k, coff:coff + ncols], start=(k == 0), stop=(k == 7)),
                      [r_hT[i], wr], [rp])
                return pt, rp

            m_x = mark()
            xt_ring = [A([128, DM], F32, f"xt{i}") for i in range(2)]
            xt_r = [Res(), Res()]
            for i in range(NT):
                xt, rx = xt_ring[i % 2], xt_r[i % 2]
                fw.dma("sync", dx[i % 2], xt[:], x_src[i * 128:(i + 1) * 128, :], reads=[r_xd], writes=[rx])
                norm_mod_tile(xt, [rx], l, b, 1, 0, hT, [r_hT[i]], i)
            release(m_x)
            dump("hT", hT[:], r_hT)
            if stop_after == "norm1":
                raise _Stop()

            m_ret = mark()
            QTr = A([64, 4, S], BF16, "QTr")
            KTr = A([64, 4, S], BF16, "KTr")
            kdec = A([128, NT, 256], BF16, "kdec")
            vtm = A([128, NT, 256], BF16, "vtm")
            sgt = A([128, NT, 256], BF16, "sgt")
            r_ret = [Res() for _ in range(NT)]
            wb, wr = load_w(w_in_cols(O_RQ, 512), 512)
            nxt_ = proj_tm(0, wb, wr, 512)
            for i in range(NT):
                pt, rp = nxt_
                nxt_ = proj_tm(i + 1, wb, wr, 512) if i + 1 < NT else None
                qk, rqk = wbf.next()
                rope(pt[:].rearrange("p (h d) -> p h d", h=8), qk[:].rearrange("p (h d) -> p h d", h=8), 8, cosT[:, i, :], sinT[:, i, :], [rp, r_tab], [rqk])
                V(lambda e, qk=qk, i=i: e.tensor_tensor(kdec[:, i, :].rearrange("p (h d) -> p h d", h=4), qk[:, 256:512].rearrange("p (h d) -> p h d", h=4),
                                                        CF("deck").unsqueeze(2).to_broadcast([128, 4, 64]), ALU.mult), [rqk, r_c], [r_ret[i]])

                def dst(pt2, rp2, i=i):
                    V(lambda e: e.tensor_scalar(QTr[:, :, i * 128:(i + 1) * 128], pt2[0:64, 0:4, :], 0.125, None, op0=ALU.mult), [rp2], [r_ret[i]])
                    V(lambda e: e.tensor_copy(KTr[:, :, i * 128:(i + 1) * 128], pt2[0:64, 4:8, :]), [rp2], [r_ret[i]])
                transposes([qk[:, n_ * 64:(n_ + 1) * 64] for n_ in range(8)], [rqk], dst, rows=64)
            if stop_after == "t1x":
                pt, rp = pf.next()
                T(lambda e, pt=pt: e.matmul(pt[:, 0:128], ident_b, ident_b, start=True, stop=True), [r_c], [rp])
                dm, rdm = wf.next()
                V(lambda e, pt=pt, dm=dm: e.tensor_copy(dm[:, 0:128], pt[:, 0:128]), [rp], [rdm])
                SC(lambda e, dm=dm: e.activation(dm[:, 128:256], dm[:, 0:128], AF.Copy), [rdm], [rdm])
                raise _Stop()
            if stop_after == "t1":
                raise _Stop()
            wb, wr = load_w(w_in_cols(O_RV, 512), 512)
            nxt_ = proj_tm(0, wb, wr, 512)
            for i in range(NT):
                pt, rp = nxt_
                nxt_ = proj_tm(i + 1, wb, wr, 512) if i + 1 < NT else None
                V(lambda e, pt=pt, i=i: e.tensor_copy(vtm[:, i, :], pt[:, 0:256]), [rp], [r_ret[i]])
                SC(lambda e, pt=pt, i=i: e.activation(sgt[:, i, :], pt[:, 256:512], AF.Silu), [rp], [r_ret[i]])
            if stop_after == "t2":
                raise _Stop()
            wb, wr = load_w(w_in_cols(O_GU, 512), 512)
            nxt_ = proj_tm(0, wb, wr, 512)
            for i in range(NT):
                pt, rp = nxt_
                nxt_ = proj_tm(i + 1, wb, wr, 512) if i + 1 < NT else None
                gu, rgu = wf.next()
                ss, rss = ws.next()
                SC(lambda e, pt=pt, gu=gu: e.activation(gu[:, 0:256], pt[:, 0:256], AF.Gelu_apprx_tanh), [rp], [rgu])
                SC(lambda e, pt=pt, gu=gu, ss=ss: e.activation(gu[:, 256:512], pt[:, 256:512], AF.Gelu_apprx_tanh, accum_out=ss[:, 0:1]), [rp], [rgu, rss])
                V(lambda e, ss=ss: e.tensor_scalar(ss[:, 1:2], ss[:, 0:1], -1.0 / 256, None, op0=ALU.mult), [rss], [rss])
                sq, rsq = wf.next()
                SC(lambda e, gu=gu, sq=sq, ss=ss: e.activation(sq[:, 0:256], gu[:, 256:512], AF.Square, bias=ss[:, 1:2], accum_out=ss[:, 2:3]), [rgu, rss], [rsq, rss])
                SC(lambda e, ss=ss: e.activation(ss[:, 2:3], ss[:, 2:3], AF.Sqrt, bias=EPS, scale=1.0 / 256), [rss], [rss])
                V(lambda e, ss=ss: e.reciprocal(ss[:, 2:3], ss[:, 2:3]), [rss], [rss])
                V(lambda e, gu=gu, sq=sq, ss=ss: e.tensor_scalar(sq[:, 0:256], gu[:, 256:512], ss[:, 1:2], ss[:, 2:3], op0=ALU.add, op1=ALU.mult), [rgu, rss], [rsq])
                vn, rvn = wbf.next()
                V(lambda e, sq=sq, vn=vn: e.tensor_tensor(vn[:, 0:256], sq[:, 0:256], gln_bc[:, l, :], ALU.mult), [rsq, r_small], [rvn])
                ps, rps = pf.next()
                for g in range(4):
                    T(lambda e, g=g, ps=ps, vn=vn: e.matmul(ps[:, g * 64:(g + 1) * 64], WTg[:, l, g, :], vn[:, g * 64:(g + 1) * 64], start=True, stop=True), [rvn, r_wtg], [rps])
                V(lambda e, ps=ps, sq=sq: e.tensor_tensor(sq[:, 256:512].rearrange("p (g c) -> p g c", g=4), ps[:, 0:256].rearrange("p (g c) -> p g c", g=4),
                                                          bTg[:, l, :].unsqueeze(2).to_broadcast([128, 4, 64]), ALU.add), [rps, r_wtg], [rsq])
                yg, ryg = wbf.next()
                V(lambda e, sq=sq, gu=gu, yg=yg: e.tensor_tensor(yg[:, 0:256], sq[:, 256:512], gu[:, 0:256], ALU.mult), [rsq, rgu], [ryg])

                def dst(pt2, rp2, i=i):
                    V(lambda e: e.tensor_copy(yT[:, 2:4, i * 128:(i + 1) * 128], pt2[:, 0:2, :]), [rp2], [r_yT[i]])
                transposes([yg[:, 0:128], yg[:, 128:256]], [ryg], dst)
            dump("QTr", QTr[:], r_ret)
            dump("KTr", KTr[:], r_ret)
            dump("vtm", vtm[:], r_ret)
            dump("sgt", sgt[:], r_ret)
            dump("kdec", kdec[:], r_ret)
            if stop_after == "t3":
                raise _Stop()
            stf = A([64, 4, 64], F32, "stf")
            stb = A([64, 4, 64], BF16, "stb")
            r_st = Res()
            V(lambda e: e.memset(stf[:], 0.0), [], [r_st])
            V(lambda e: e.memset(stb[:], 0.0), [], [r_st])
            for i in range(NT):
                cs = slice(i * 128, (i + 1) * 128)
                pS, rpS = pf.next()
                for h in range(4):
                    T(lambda e, h=h, pS=pS: e.matmul(pS[:, h * 128:(h + 1) * 128], KTr[:, h, cs], QTr[:, h, cs], start=True, stop=True), [r_ret[i]], [rpS])
                PT, rPT = wbf.next()
                V(lambda e, pS=pS, PT=PT: e.tensor_tensor(PT[:], pS[:], CF("decT"), ALU.mult), [rpS, r_c], [rPT])
                if stop_after in ("r1", "r1a"):
                    raise _Stop()
                pO, rpO = pf.next()
                for h in range(4):
                    pr, hp = h // 2, (h % 2) * 64
                    T(lambda e, h=h, pO=pO, PT=PT: e.matmul(pO[:, h * 64:(h + 1) * 64], PT[:, h * 128:(h + 1) * 128], vtm[:, i, h * 64:(h + 1) * 64], start=True, stop=True), [rPT, r_ret[i]], [rpO])
                    T(lambda e, h=h, pO=pO: e.matmul(pO[:, 256 + h * 64:256 + (h + 1) * 64], QTr[:, h, cs], stb[:, h, :], start=True, stop=True), [r_ret[i], r_st], [rpO])
                o, ro = wf.next()
                V(lambda e, pO=pO, o=o: e.tensor_tensor(o[:, 256:512].rearrange("p (h d) -> p h d", h=4), pO[:, 256:512].rearrange("p (h d) -> p h d", h=4),
                                                        CF("decq").unsqueeze(2).to_broadcast([128, 4, 64]), ALU.mult), [rpO, r_c], [ro])
                V(lambda e, pO=pO, o=o: e.tensor_tensor(o[:, 0:256], o[:, 256:512], pO[:, 0:256], ALU.add), [rpO, ro], [ro])
                if stop_after == "r2":
                    raise _Stop()
                pK, rpK = pf.next()
                for h in range(4):
                    T(lambda e, h=h, pK=pK: e.matmul(pK[0:64, h * 64:(h + 1) * 64], kdec[:, i, h * 64:(h + 1) * 64], vtm[:, i, h * 64:(h + 1) * 64], start=True, stop=True), [r_ret[i]], [rpK])
                for h in range(4):
                    V(lambda e, h=h, pK=pK: e.scalar_tensor_tensor(stf[:, h, :], stf[:, h, :], _DCHUNK[h], pK[0:64, h * 64:(h + 1) * 64],
                                                                   op0=ALU.mult, op1=ALU.add), [rpK, r_st], [r_st])
                V(lambda e: e.tensor_copy(stb[:], stf[:]), [r_st], [r_st])
                if stop_after == "r3":
                    raise _Stop()
                on, ron = wf.next()
                headnorm(o[:, 0:256].rearrange("p (h d) -> p h d", h=4), 4, gret_bc[:, l, :].rearrange("p (h d) -> p h d", h=4), on[:, 0:256].rearrange("p (h d) -> p h d", h=4), [ro], [ron])
                yr, ryr = wbf.next()
                V(lambda e, on=on, yr=yr, i=i: e.tensor_tensor(yr[:, 0:256], on[:, 0:256], sgt[:, i, :], ALU.mult), [ron, r_ret[i]], [ryr])

                def dst(pt2, rp2, i=i):
                    V(lambda e: e.tensor_copy(yT[:, 0:2, i * 128:(i + 1) * 128], pt2[:, 0:2, :]), [rp2], [r_yT[i]])
                transposes([yr[:, 0:128], yr[:, 128:256]], [ryr], dst)
            if stop_after == "ret":
                raise _Stop()
            release(m_ret)
            m_nsa = mark()
            KsT = A([64, 2, S], BF16, "KsT")
            KwT = A([64, 2, S], BF16, "KwT")
            kcr = A([64, 2, S], BF16, "kcr")
            vcr = A([64, 2, S], BF16, "vcr")
            Vs = A([128, NT, 2, 66], BF16, "Vs")
            Vw = A([128, NT, 2, 66], BF16, "Vw")
            gat = A([128, NT, 24], F32, "gat")
            KcT = A([64, 2, 128], BF16, "KcT")
            Vca = A([128, 2, 98], BF16, "Vca")
            r_nsa = [Res() for _ in range(NT)]
            m_cmp = mark()
            w1 = [A([64, 32, 128], BF16, f"w1{i}") for i in range(2)]
            w2 = [A([128, 64], BF16, f"w2{i}") for i in range(2)]
            peT = A([64, 2, 32], BF16, "peT")
            bpe = A([128, 2], F32, "bpe")
            r_raw = [Res() for _ in range(4)]
            r_cw = Res()
            r_cmp = Res()
            for wi, (n1, n2, npe) in enumerate((("cmp_w1_k", "cmp_w2_k", "cmp_pe_k"), ("cmp_w1_v", "cmp_w2_v", "cmp_pe_v"))):
                fw.dma("gpsimd", dmisc, w1[wi][:], W[n1][l].rearrange("l d h -> d l h"), writes=[r_cw])
                fw.dma("gpsimd", dmisc, w2[wi][:], W[n2][l], writes=[r_cw])
                fw.dma("gpsimd", dmisc, peT[0:64, wi, :], W[npe][l].rearrange("l d -> d l"), writes=[r_cw])
            fw.group_end(dmisc, [r_cw])
            V(lambda e: e.memset(Vs[:, :, :, 64:66], 1.0), [], r_nsa)
            V(lambda e: e.memset(Vw[:, :, :, 64:66], 1.0), [], r_nsa)
            V(lambda e: e.memset(Vca[:, :, 64:98], 1.0), [], [r_cmp])
            for g in range(2):
                V(lambda e, g=g: e.tensor_copy(Vca[:, g, 65:97], CB("ovl")), [r_c], [r_cmp])
            wb, wr = load_w(w_in_cols(O_KC, 256), 256)
            for tb in range(4):
                for wi, dstb in enumerate((kcr, vcr)):
                    for g in range(2):
                        pt, rp = pf.next()
                        for k in range(8):
                            T(lambda e, k=k, pt=pt, wi=wi, g=g: e.matmul(pt[0:64, :], wb[:, k, wi * 128 + g * 64:wi * 128 + (g + 1) * 64], hT[:, k, tb * 512:(tb + 1) * 512], start=(k == 0), stop=(k == 7)),
                              r_hT[tb * 4:tb * 4 + 4] + [wr], [rp])
                        SC(lambda e, pt=pt, dstb=dstb, g=g: e.activation(dstb[:, g, tb * 512:(tb + 1) * 512], pt[0:64, :], AF.Copy), [rp], [r_raw[tb]])
            dump("kcr", kcr[:], r_raw)
            if stop_after == "c0":
                raise _Stop()
            pbias, rpb = pf.next()
            for wi in range(2):
                for l_ in range(32):
                    T(lambda e, wi=wi, l_=l_: e.matmul(pbias[:, wi:wi + 1], w1[wi][0:64, l_, :], peT[0:64, wi, l_:l_ + 1], start=(l_ == 0), stop=(l_ == 31)), [r_cw], [rpb])
            V(lambda e: e.tensor_copy(bpe[:], pbias[:, 0:2]), [rpb], [r_cmp])
            dump("bpe", bpe[:], [r_cmp])
            if stop_after == "c1":
                raise _Stop()
            for wi, raw in enumerate((kcr, vcr)):
                ph, rph = pf.next()
                for g in range(2):
                    for l_ in range(32):
                        T(lambda e, g=g, l_=l_, wi=wi, raw=raw, ph=ph: e.matmul(ph[:, g * 128:g * 128 + 127], w1[wi][:, l_, :], raw[:, g, l_:l_ + 16 * 126 + 1:16],
                                                                                 start=(l_ == 0), stop=(l_ == 31)), r_raw + [r_cw], [rph])
                hb, rhb = wbf.next()
                V(lambda e, hb=hb: e.memset(hb[:, 0:256].rearrange("p (g n) -> p g n", g=2)[:, :, 127:128], 0.0), [], [rhb])
                for g in range(2):
                    SC(lambda e, ph=ph, hb=hb, wi=wi, g=g: e.activation(hb[:, g * 128:g * 128 + 127], ph[:, g * 128:g * 128 + 127],
                                                                     AF.Gelu_apprx_tanh, bias=bpe[:, wi:wi + 1]), [rph, r_cmp], [rhb])
                pk, rpk = pf.next()
                for g in range(2):
                    T(lambda e, g=g, pk=pk, hb=hb, wi=wi: e.matmul(pk[:, g * 64:(g + 1) * 64], hb[:, g * 128:(g + 1) * 128], w2[wi][:], start=True, stop=True), [rhb, r_cw], [rpk])
                if stop_after == "c2":
                    raise _Stop()
                if wi == 0:
                    kn, rkn = wf.next()
                    headnorm(pk[:, 0:128].rearrange("p (h d) -> p h d", h=2), 2, gk_bc[:, l, 0, :].unsqueeze(1).to_broadcast([128, 2, 64]),
                             kn[:, 0:128].rearrange("p (h d) -> p h d", h=2), [rpk], [rkn])
                    kr, rkr = wbf.next()
                    rope(kn[:, 0:128].rearrange("p (h d) -> p h d", h=2), kr[:, 0:128].rearrange("p (h d) -> p h d", h=2), 2, cosC[:, :], sinC[:, :], [rkn, r_tab], [rkr])

                    def dstk(pt2, rp2):
                        V(lambda e: e.tensor_copy(KcT[:, :, :], pt2[0:64, 0:2, :]), [rp2], [r_cmp])
                    transposes([kr[:, 0:64], kr[:, 64:128]], [rkr], dstk, rows=64)
                else:
                    V(lambda e, pk=pk: e.tensor_copy(Vca[:, :, 0:64], pk[:, 0:128].rearrange("p (g d) -> p g d", g=2)), [rpk], [r_cmp])
            dump("KcT", KcT[:], [r_cmp])
            if stop_after == "cmp":
                raise _Stop()
            release(m_cmp)
            QTn = A([64, 2, 4, S], BF16, "QTn")
            dump("Vca", Vca[:], [r_cmp])
            wb, wr = load_w(w_in_cols(O_NQ, 512), 512)
            nxt_ = proj_tm(0, wb, wr, 512)
            for i in range(NT):
                pt, rp = nxt_
                nxt_ = proj_tm(i + 1, wb, wr, 512) if i + 1 < NT else None
                qn, rqn = wf.next()
                headnorm(pt[:].rearrange("p (h d) -> p h d", h=8), 8, gq_bc[:, l, :].unsqueeze(1).to_broadcast([128, 8, 64]), qn[:].rearrange("p (h d) -> p h d", h=8), [rp], [rqn])
                qr, rqr = wbf.next()
                rope(qn[:].rearrange("p (h d) -> p h d", h=8), qr[:].rearrange("p (h d) -> p h d", h=8), 8, cosT[:, i, :], sinT[:, i, :], [rqn, r_tab], [rqr])

                def dst(pt2, rp2, i=i):
                    for g in range(2):
                        V(lambda e, g=g: e.tensor_copy(QTn[:, g, :, i * 128:(i + 1) * 128], pt2[0:64, g * 4:(g + 1) * 4, :]), [rp2], [r_nsa[i]])
                transposes([qr[:, n_ * 64:(n_ + 1) * 64] for n_ in range(8)], [rqr], dst, rows=64)
            if stop_after == "p4":
                raise _Stop()
            wb, wr = load_w(w_in_cols(O_KS, 512), 512)
            nxt_ = proj_tm(0, wb, wr, 512)
            for i in range(NT):
                pt, rp = nxt_
                nxt_ = proj_tm(i + 1, wb, wr, 512) if i + 1 < NT else None
                kn, rkn = wf.next()
                kr, rkr = wbf.next()
                for which, c0 in ((1, 0), (2, 256)):
                    o0 = 0 if which == 1 else 128
                    headnorm(pt[:, c0:c0 + 128].rearrange("p (h d) -> p h d", h=2), 2, gk_bc[:, l, which, :].unsqueeze(1).to_broadcast([128, 2, 64]),
                             kn[:, o0:o0 + 128].rearrange("p (h d) -> p h d", h=2), [rp], [rkn])
                rope(kn[:, 0:256].rearrange("p (h d) -> p h d", h=4), kr[:, 0:256].rearrange("p (h d) -> p h d", h=4), 4, cosT[:, i, :], sinT[:, i, :], [rkn, r_tab], [rkr])
                V(lambda e, pt=pt, i=i: e.tensor_copy(Vs[:, i, :, 0:64], pt[:, 128:256].rearrange("p (g d) -> p g d", g=2)), [rp], [r_nsa[i]])
                V(lambda e, pt=pt, i=i: e.tensor_copy(Vw[:, i, :, 0:64], pt[:, 384:512].rearrange("p (g d) -> p g d", g=2)), [rp], [r_nsa[i]])

                def dst(pt2, rp2, i=i):
                    V(lambda e: e.tensor_copy(KsT[:, :, i * 128:(i + 1) * 128], pt2[0:64, 0:2, :]), [rp2], [r_nsa[i]])
                    V(lambda e: e.tensor_copy(KwT[:, :, i * 128:(i + 1) * 128], pt2[0:64, 2:4, :]), [rp2], [r_nsa[i]])
                transposes([kr[:, n_ * 64:(n_ + 1) * 64] for n_ in range(4)], [rkr], dst, rows=64)
            if stop_after == "p5":
                raise _Stop()
            wb, wr = load_w(w_in_cols(O_GL, 24), 24)
            nxt_ = proj_tm(0, wb, wr, 24)
            for i in range(NT):
                pt, rp = nxt_
                nxt_ = proj_tm(i + 1, wb, wr, 24) if i + 1 < NT else None
                SC(lambda e, pt=pt, i=i: e.activation(gat[:, i, :], pt[:, 0:24], AF.Sigmoid), [rp], [r_nsa[i]])
            dump("QTn", QTn[:], r_nsa)
            dump("KsT", KsT[:], r_nsa)
            dump("gat", gat[:], r_nsa)
            if stop_after == "nsaproj":
                raise _Stop()
            nselT = [A([128, 128], BF16, f"nselT{g}") for g in range(2)]
            r_nsel = [Res(), Res()]
            nsel_ring = Ring([A([128, 32], BF16, f"nsel{q_}") for q_ in range(2)])
            for i in range(NT):
                cs = slice(i * 128, (i + 1) * 128)
                for g in range(2):
                    QT4 = QTn[:, g, :, cs]
                    pC, rpC = paccA.next()
                    pOw, rpOw = paccA.next()
                    pOs, rpOs = paccA.next()
                    pC3 = pC[:, 0:388].rearrange("p (r c) -> p r c", r=4)
                    jlo = max(0, i - 4)
                    pairs = [("c", 0)] + [("w", j) for j in range(jlo, i + 1)] + [("s", i)] + [("s", j) for j in range(i)]
                    first = {"w": jlo, "s": i}
                    last = {"w": i, "s": (i - 1 if i > 0 else i)}
                    state = {}

                    def emit_nsel_transpose():
                        nsel, rns = state["nsel"]
                        pt2, rp2 = pb.next()
                        T(lambda e: e.transpose(pt2[0:32, 0, :], nsel[:, 0:32], ident_b), [rns, r_c], [rp2])
                        V(lambda e: e.tensor_copy(nselT[g][0:32, :], pt2[0:32, 0, :]), [rp2], [r_nsel[g]])

                    def emit_S(kind, j):
                        pS, rpS = pfA.next()
                        pS3 = pS[:].rearrange("p (r q) -> p r q", r=4)
                        if kind == "c":
                            T(lambda e: e.matmul(pS3, KcT[:, g, :], QT4, start=True, stop=False), [r_cmp, r_nsa[i]], [rpS])
                            T(lambda e: e.matmul(pS3, ident_b, CB("cmaskT")[:, cs].unsqueeze(1).to_broadcast([128, 4, 128]), start=False, stop=True), [r_c], [rpS])
                            return pS, rpS
                        KT_ = KsT if kind == "s" else KwT
                        mk = None
                        if j == i:
                            mk = (ident_b, CB("causT").unsqueeze(1).to_broadcast([128, 4, 128]), [r_c])
                        elif kind == "s":
                            if "nselT_done" not in state:
                                emit_nsel_transpose()
                                state["nselT_done"] = True
                            mk = (CB("Esel", 32)[:, j * 128:(j + 1) * 128], nselT[g][0:32, :].unsqueeze(1).to_broadcast([32, 4, 128]), [r_c, r_nsel[g]])
                        elif j == i - 4:
                            mk = (ident_b, CB("antiT").unsqueeze(1).to_broadcast([128, 4, 128]), [r_c])
                        T(lambda e: e.matmul(pS3, KT_[:, g, j * 128:(j + 1) * 128], QT4, start=True, stop=(mk is None)), [r_nsa[j], r_nsa[i]], [rpS])
                        if mk is not None:
                            T(lambda e: e.matmul(pS3, mk[0], mk[1], start=False, stop=True), mk[2], [rpS])
                        return pS, rpS

                    def emit_PV(kind, j, pS, rpS):
                        Pm, rPm = wbf.next()
                        SC(lambda e: e.activation(Pm[:], pS[:], AF.Exp), [rpS], [rPm])
                        if kind == "c":
                            for r in range(4):
                                T(lambda e: e.matmul(pC[:, r * 97:(r + 1) * 97], Pm[:, r * 128:(r + 1) * 128], Vca[:, g, 0:97], start=(r == 0), stop=(r == 3)), [rPm, r_cmp], [rpC])
                            return
                        pO, rpO, Vt_ = (pOs, rpOs, Vs) if kind == "s" else (pOw, rpOw, Vw)
                        for r in range(4):
                            T(lambda e: e.matmul(pO[:, r * 65:(r + 1) * 65], Pm[:, r * 128:(r + 1) * 128], Vt_[:, j, g, 0:65], start=(j == first[kind] and r == 0), stop=(j == last[kind] and r == 3)),
                              [rPm, r_nsa[j]], [rpO])

                    def emit_nsel_chain():
                        den, rden = ws.next()
                        state["den"] = (den, rden)
                        den3 = den[:, 0:12].rearrange("p (b r) -> p b r", b=3)
                        V(lambda e: e.tensor_scalar(den3[:, 0, :], pC3[:, :, 64], 1e-30, None, op0=ALU.max), [rpC], [rden])
                        rd0, rrd0 = ws.next()
                        V(lambda e: e.reciprocal(rd0[:, 0:4], den3[:, 0, :]), [rden], [rrd0])
                        sc, rsc = ws.next()
                        V(lambda e: e.tensor_scalar(sc[:, 0:32], pC3[:, 0, 65:97], rd0[:, 0:1], None, op0=ALU.mult), [rpC, rrd0], [rsc])
                        for r in range(1, 4):
                            V(lambda e: e.scalar_tensor_tensor(sc[:, 0:32], pC3[:, r, 65:97], rd0[:, r:r + 1], sc[:, 0:32], op0=ALU.mult, op1=ALU.add), [rpC, rrd0, rsc], [rsc])
                        V(lambda e: e.tensor_tensor(sc[:, 0:32], sc[:, 0:32], CF("a01")[:, i * 32:(i + 1) * 32], ALU.mult), [rsc, r_c], [rsc])
                        V(lambda e: e.tensor_tensor(sc[:, 0:32], sc[:, 0:32], CF("addc")[:, i * 32:(i + 1) * 32], ALU.add), [rsc, r_c], [rsc])
                        t8, rt8 = ws.next()
                        V(lambda e: e.max(out=t8[:, 0:8], in_=sc[:, 0:32]), [rsc], [rt8])
                        nsel, rns = nsel_ring.next()
                        V(lambda e: e.tensor_scalar(nsel[:, 0:32], sc[:, 0:32], t8[:, 7:8], 1.0, op0=ALU.is_ge, op1=ALU.subtract), [rsc, rt8], [rns])
                        state["nsel"] = (nsel, rns)

                    cur = emit_S(*pairs[0])
                    for kk, (kind, j) in enumerate(pairs):
                        nxt = emit_S(*pairs[kk + 1]) if kk + 1 < len(pairs) else None
                        emit_PV(kind, j, *cur)
                        if kind == "c":
                            emit_nsel_chain()
                        cur = nxt
                    den, rden = state["den"]
                    den3 = den[:, 0:12].rearrange("p (b r) -> p b r", b=3)
                    outs = [(pOs, rpOs), (pOw, rpOw)]
                    pO3s = [pC3] + [po[:, 0:260].rearrange("p (r c) -> p r c", r=4) for po, _ in outs]
                    rall = [rpC] + [rr_ for _, rr_ in outs]
                    for br in (1, 2):
                        V(lambda e, br=br, den3=den3: e.tensor_scalar(den3[:, br, :], pO3s[br][:, :, 64], 1e-30, None, op0=ALU.max), [rall[br]], [rden])
                    cf_, rcf = ws.next()
                    cf3 = cf_[:, 0:12].rearrange("p (b r) -> p b r", b=3)
                    V(lambda e, cf_=cf_, den=den: e.reciprocal(cf_[:, 0:12], den[:, 0:12]), [rden], [rcf])
                    V(lambda e, cf3=cf3: e.tensor_tensor(cf3, cf3, gat[:, i, g * 12:(g + 1) * 12].rearrange("p (r b) -> p b r", b=3), ALU.mult), [rcf, r_nsa[i]], [rcf])
                    oa, roa = wf.next()
                    ob, rob = wf.next()
                    oa3 = oa[:, 0:256].rearrange("p (r d) -> p r d", r=4)
                    ob3 = ob[:, 0:256].rearrange("p (r d) -> p r d", r=4)
                    V(lambda e, oa3=oa3, cf3=cf3: e.tensor_tensor(oa3, pO3s[0][:, :, 0:64], cf3[:, 0, :].unsqueeze(2).to_broadcast([128, 4, 64]), ALU.mult), [rall[0], rcf], [roa])
                    for br in (1, 2):
                        V(lambda e, br=br, ob3=ob3, cf3=cf3: e.tensor_tensor(ob3, pO3s[br][:, :, 0:64], cf3[:, br, :].unsqueeze(2).to_broadcast([128, 4, 64]), ALU.mult), [rall[br], rcf], [rob])
                        if br == 1:
                            V(lambda e, oa=oa, ob=ob: e.tensor_tensor(oa[:, 0:256], oa[:, 0:256], ob[:, 0:256], ALU.add), [roa, rob], [roa])
                    yn, ryn = wbf.next()
                    V(lambda e, oa=oa, ob=ob, yn=yn: e.tensor_tensor(yn[:, 0:256], oa[:, 0:256], ob[:, 0:256], ALU.add), [roa, rob], [ryn])

                    def dst(pt3, rp3, i=i, g=g):
                        V(lambda e: e.tensor_copy(yT[:, 4 + 2 * g:6 + 2 * g, i * 128:(i + 1) * 128], pt3[:, 0:2, :]), [rp3], [r_yT[i]])
                    transposes([yn[:, 0:128], yn[:, 128:256]], [ryn], dst)
            dump("yT", yT[:], r_yT)
            if stop_after == "nsa":
                raise _Stop()
            release(m_nsa)
            release(m_layer)
            m_b = mark()
            xres = A([128, NT, DM], F32, "xres")
            r_x = [Res() for _ in range(NT)]
            gbc = A([128, 1, DM], F32, "gbc")
            r_gbc = Res()
            gates = A([128, NT, 16], F32, "gates")
            r_gates = [Res() for _ in range(NT)]
            wg = [A([128, 8, 512], BF16, f"wg{i}") for i in range(2)]
            wu = [A([128, 8, 512], BF16, f"wu{i}") for i in range(2)]
            wd = [A([128, 4, 1024], BF16, f"wd{i}") for i in range(2)]
            r_we = [Res(), Res()]
            AT = [A([128, 4, 512], BF16, f"AT{i}") for i in range(2)]
            r_AT = [Res(), Res()]
            xt2 = [A([128, DM], F32, f"xtb{i}") for i in range(1)] * 2
            xt2_r = [Res()] * 2
            wout = wg[0], wg[1]
            def make_gbc(wh):
                vi = 0
                for half in range(2):
                    pt, rp = pf.next()
                    for kk in range(4):
                        k = half * 4 + kk
                        dg, rdg = wf.next()
                        V(lambda e, dg=dg, k=k, wh=wh: e.tensor_scalar(dg[:, 0:128], ident_f, modT[:, l, wh, k, b:b + 1], None, op0=ALU.mult), [r_c, r_mod], [rdg])
                        T(lambda e, dg=dg, pt=pt, kk=kk: e.matmul(pt[:, kk * 128:(kk + 1) * 128], CF("ones_f"), dg[:, 0:128], start=True, stop=True), [rdg, r_c], [rp])
                    V(lambda e, pt=pt, vi=vi, half=half: e.tensor_copy(gbc[:, vi, half * 512:(half + 1) * 512], pt[:]), [rp], [r_gbc])
            make_gbc(2)
            for half in range(2):
                fw.dma("gpsimd", dmoe[half], wg[half][:], W["w_out"][l, :, half * 512:(half + 1) * 512].rearrange("(k p) n -> p k n", p=128), writes=[r_we[half]])
            for i in range(NT):
                cs = slice(i * 128, (i + 1) * 128)
                xt, rxt = xt2[i % 2], xt2_r[i % 2]
                fw.dma("sync", dx[i % 2], xt[:], x_src[cs, :], reads=[r_xd], writes=[rxt])
                for half in range(2):
                    pt, rp = pf.next()
                    for k in range(8):
                        T(lambda e, k=k, pt=pt, half=half: e.matmul(pt[:], yT[:, k, cs], wg[half][:, k, :], start=(k == 0), stop=(k == 7)), [r_yT[i], r_we[half]], [rp])
                    tm, rtm = wf.next()
                    V(lambda e, pt=pt, tm=tm, half=half: e.tensor_tensor(tm[:], pt[:], gbc[:, 0, half * 512:(half + 1) * 512], ALU.mult), [rp, r_gbc], [rtm])
                    V(lambda e, tm=tm, xt=xt, half=half, i=i: e.tensor_tensor(xres[:, i, half * 512:(half + 1) * 512], tm[:], xt[:, half * 512:(half + 1) * 512], ALU.add), [rtm, rxt], [r_x[i]])
            dump("xmid", xres[:], r_x)
            if stop_after == "wout":
                raise _Stop()
            make_gbc(5)
            prl, rprl = pacc.next()
            for i in range(NT):
                cs = slice(i * 128, (i + 1) * 128)
                norm_mod_tile(xres[:, i, :], [r_x[i]], l, b, 4, 3, yT, [r_yT[i]], i)
                for k in range(8):
                    T(lambda e: e.matmul(prl[:, i * 16:(i + 1) * 16], yT[:, k, cs], rw_sb[:, k, :], start=(k == 0), stop=(k == 7)), [r_yT[i], r_rw], [rprl])
            tE, rE = wf.next()
            tS, rS = wf.next()
            tA, rA = wf.next()
            tB, rB = wf.next()
            tm, rm = wf.next()
            L3 = prl[:, 0:256].rearrange("p (t e) -> p t e", t=NT)
            E3 = tE[:, 0:256].rearrange("p (t e) -> p t e", t=NT)
            S3 = tS[:, 0:256].rearrange("p (t e) -> p t e", t=NT)
            S4 = tS[:, 0:256].rearrange("p (tg e) -> p tg e", e=4)
            A3 = tA[:, 0:256].rearrange("p (t e) -> p t e", t=NT)
            A4 = tA[:, 0:256].rearrange("p (tg e) -> p tg e", e=4)
            B3 = tB[:, 0:256].rearrange("p (t e) -> p t e", t=NT)
            mx = tm[:, 0:16]
            se = tm[:, 16:32]
            m1 = tm[:, 64:128]
            m2 = tm[:, 128:192]
            gmx = tm[:, 32:48]
            t1 = tm[:, 192:208]
            t2 = tm[:, 208:224]
            s2 = tm[:, 224:240]
            bc16 = lambda ap: ap.unsqueeze(2).to_broadcast([128, NT, 16])
            V(lambda e: e.tensor_reduce(mx, L3, AX.X, ALU.max), [rprl], [rm])
            V(lambda e: e.tensor_tensor(E3, L3, bc16(mx), ALU.subtract), [rprl, rm], [rE])
            SC(lambda e: e.activation(tE[:, 0:256], tE[:, 0:256], AF.Exp), [rE], [rE])
            V(lambda e: e.tensor_reduce(se, E3, AX.X, ALU.add), [rE], [rm])
            V(lambda e: e.reciprocal(se, se), [rm], [rm])
            V(lambda e: e.tensor_tensor(E3, E3, bc16(se), ALU.mult), [rE, rm], [rE])
            V(lambda e: e.tensor_tensor(S3, E3, rb_bc[:].unsqueeze(1).to_broadcast([128, NT, 16]), ALU.add), [rE, r_small], [rS])
            V(lambda e: e.tensor_reduce(m1, S4, AX.X, ALU.max), [rS], [rm])
            V(lambda e: e.tensor_tensor(A4, S4, m1.unsqueeze(2).to_broadcast([128, 64, 4]), ALU.is_equal), [rS, rm], [rA])
            V(lambda e: e.scalar_tensor_tensor(tA[:, 0:256], tA[:, 0:256], -1e9, tS[:, 0:256], op0=ALU.mult, op1=ALU.add), [rA, rS], [rA])
            V(lambda e: e.tensor_reduce(m2, A4, AX.X, ALU.max), [rA], [rm])
            V(lambda e: e.tensor_tensor(m1, m1, m2, ALU.add), [rm], [rm])
            V(lambda e: e.tensor_reduce(gmx, m1.rearrange("p (t g) -> p t g", g=4), AX.X, ALU.max), [rm], [rm])
            V(lambda e: e.tensor_tensor(m2.rearrange("p (t g) -> p t g", g=4), m1.rearrange("p (t g) -> p t g", g=4), gmx.unsqueeze(2).to_broadcast([128, NT, 4]), ALU.is_ge), [rm], [rm])
            V(lambda e: e.tensor_scalar(m2, m2, 1e9, -1e9, op0=ALU.mult, op1=ALU.add), [rm], [rm])
            V(lambda e: e.tensor_tensor(A4, S4, m2.unsqueeze(2).to_broadcast([128, 64, 4]), ALU.add), [rS, rm], [rA])
            V(lambda e: e.tensor_reduce(t1, A3, AX.X, ALU.max), [rA], [rm])
            V(lambda e: e.tensor_tensor(B3, A3, bc16(t1), ALU.is_equal), [rA, rm], [rB])
            V(lambda e: e.scalar_tensor_tensor(tB[:, 0:256], tB[:, 0:256], -1e9, tA[:, 0:256], op0=ALU.mult, op1=ALU.add), [rB, rA], [rB])
            V(lambda e: e.tensor_reduce(t2, B3, AX.X, ALU.max), [rB], [rm])
            V(lambda e: e.tensor_tensor(B3, A3, bc16(t2), ALU.is_ge), [rA, rm], [rB])
            V(lambda e: e.tensor_tensor(tB[:, 0:256], tB[:, 0:256], tE[:, 0:256], ALU.mult), [rB, rE], [rB])
            V(lambda e: e.tensor_reduce(s2, B3, AX.X, ALU.add), [rB], [rm])
            V(lambda e: e.reciprocal(s2, s2), [rm], [rm])
            V(lambda e: e.tensor_tensor(gates[:], B3, bc16(s2), ALU.mult), [rB, rm], r_gates)
            dump("gates", gates[:], r_gates)
            dump("h2T", yT[:], r_yT)
            if stop_after == "router":
                raise _Stop()
            for ex in range(NE if not dbg.get("_moe_experts") else dbg["_moe_experts"][0][0]):
                sl = ex % 2
                fw.dma("gpsimd", dmoe[sl], wg[sl][:], W["moe_w_gate"][l, ex].rearrange("(k p) n -> p k n", p=128), writes=[r_we[sl]])
                fw.dma("gpsimd", dmoe[sl], wu[sl][:], W["moe_w_up"][l, ex].rearrange("(k p) n -> p k n", p=128), writes=[r_we[sl]])
                fw.dma("gpsimd", dmoe[sl], wd[sl][:], W["moe_w_down"][l, ex].rearrange("(k p) n -> p k n", p=128), writes=[r_we[sl]])
                G(lambda e, sl=sl: e.tensor_tensor(wd[sl][:], wd[sl][:], gbc[:, 0, :].unsqueeze(1).to_broadcast([128, 4, DM]), ALU.mult), [r_we[sl], r_gbc], [r_we[sl]])
                def stage1(tb):
                    at, rat = AT[tb % 2], r_AT[tb % 2]
                    for dc in range(4):
                        pG, rpG = pf.next()
                        pU, rpU = pf.next()
                        for k in range(8):
                            T(lambda e: e.matmul(pG[:], wg[sl][:, k, dc * 128:(dc + 1) * 128], yT[:, k, tb * 512:(tb + 1) * 512], start=(k == 0), stop=(k == 7)),
                              r_yT[tb * 4:tb * 4 + 4] + [r_we[sl]], [rpG])
                        for k in range(8):
                            T(lambda e: e.matmul(pU[:], wu[sl][:, k, dc * 128:(dc + 1) * 128], yT[:, k, tb * 512:(tb + 1) * 512], start=(k == 0), stop=(k == 7)),
                              r_yT[tb * 4:tb * 4 + 4] + [r_we[sl]], [rpU])
                        sg, rsg = wf.next()
                        SC(lambda e: e.activation(sg[:], pG[:], AF.Silu), [rpG], [rsg])
                        V(lambda e: e.tensor_tensor(at[:, dc, :], sg[:], pU[:], ALU.mult), [rsg, rpU], [rat])

                def stage2(tb):
                    at, rat = AT[tb % 2], r_AT[tb % 2]
                    for tt in range(4):
                        i = tb * 4 + tt
                        for dh in range(2):
                            pO, rpO = pacc.next()
                            for dc in range(4):
                                T(lambda e: e.matmul(pO[:], at[:, dc, tt * 128:(tt + 1) * 128], wd[sl][:, dc, dh * 512:(dh + 1) * 512], start=(dc == 0), stop=(dc == 3)),
                                  [rat, r_we[sl]], [rpO])
                            V(lambda e: e.scalar_tensor_tensor(xres[:, i, dh * 512:(dh + 1) * 512], pO[:], gates[:, i, ex:ex + 1], xres[:, i, dh * 512:(dh + 1) * 512],
                                                               op0=ALU.mult, op1=ALU.add), [rpO, r_gates[i], r_x[i]], [r_x[i]])

                stage1(0)
                for tb in range(4):
                    if tb + 1 < 4:
                        stage1(tb + 1)
                    stage2(tb)
            is_last = (l == nlayers - 1)
            for i in range(NT):
                fw.dma("sync", dout, x_dst[i * 128:(i + 1) * 128, :], xres[:, i, :], reads=[r_x[i]], writes=([] if is_last else [r_xd]))
            release(m_b)

        r_xd = Res()
        for b in range(nseq):
            m_tab = mark()
            cosT = A([128, NT, 32], F32, "cos")
            sinT = A([128, NT, 32], F32, "sin")
            cosC = A([128, 32], F32, "cosC")
            sinC = A([128, 32], F32, "sinC")
            r_tab = Res()
            m_tmp = mark()
            posi = A([128, 128], I32, "posi")
            posf = A([128, 128], F32, "posf")
            post = A([128, NT + 1], F32, "post")
            r_pos = Res()
            dpos = fw.dma_sem(f"dpos{b}")
            fw.dma("sync", dpos, posi[0:NT, :], pos_d[b].rearrange("(i p) -> i p", p=128), writes=[r_pos])
            V(lambda e: e.tensor_copy(posf[0:NT, :], posi[0:NT, :]), [r_pos], [r_pos])
            pt, rp = pf.next()
            T(lambda e, pt=pt: e.transpose(pt[:, 0:NT], posf[0:NT, :], ident_f[0:NT, 0:NT]), [r_pos, r_c], [rp])
            V(lambda e, pt=pt: e.tensor_copy(post[:, 0:NT], pt[:, 0:NT]), [rp], [r_pos])
            posc = A([128, 1], I32, "posc")
            dpos2 = fw.dma_sem(f"dposc{b}")
            r_posc = Res()
            fw.dma("sync", dpos2, posc[0:127, :], pos_d[b, 31:2048:16].unsqueeze(1), writes=[r_posc])
            V(lambda e: e.memset(post[:, NT:NT + 1], 0.0), [r_pos], [r_pos])
            V(lambda e: e.tensor_copy(post[0:127, NT:NT + 1], posc[0:127, :]), [r_pos, r_posc], [r_pos])

            def make_tables(n_t, pos_ap, cos_out, sin_out):
                ang = A([128, n_t, 32], F32, "ang")
                kf = A([128, n_t, 32], F32, "kf")
                ki = A([128, n_t, 32], I32, "ki")
                rr = A([128, n_t, 32], F32, "rr")
                r_a = Res()
                V(lambda e: e.tensor_tensor(ang[:], CF("invf").unsqueeze(1).to_broadcast([128, n_t, 32]),
                                            pos_ap.unsqueeze(2).to_broadcast([128, n_t, 32]), ALU.mult), [r_pos, r_c], [r_a])
                V(lambda e: e.tensor_scalar(kf[:], ang[:], 1.0 / TWO_PI, None, op0=ALU.mult), [r_a], [r_a])
                V(lambda e: e.tensor_copy(ki[:], kf[:]), [r_a], [r_a])
                V(lambda e: e.tensor_copy(kf[:], ki[:]), [r_a], [r_a])
                V(lambda e: e.scalar_tensor_tensor(ang[:], kf[:], -TWO_PI, ang[:], op0=ALU.mult, op1=ALU.add), [r_a], [r_a])
                for shift, outp in ((0.0, sin_out), (PI / 2, cos_out)):
                    V(lambda e, shift=shift: e.tensor_scalar(rr[:], ang[:], shift, None, op0=ALU.add), [r_a], [r_a])
                    V(lambda e: e.tensor_scalar(kf[:], rr[:], PI, -TWO_PI, op0=ALU.is_gt, op1=ALU.mult), [r_a], [r_a])
                    V(lambda e: e.tensor_tensor(rr[:], rr[:], kf[:], ALU.add), [r_a], [r_a])
                    V(lambda e: e.tensor_scalar(rr[:], rr[:], PI, -PI, op0=ALU.min, op1=ALU.max), [r_a], [r_a])
                    SC(lambda e, outp=outp: e.activation(outp, rr[:], AF.Sin), [r_a], [r_tab])

            make_tables(NT, post[:, 0:NT], cosT[:], sinT[:])
            make_tables(1, post[:, NT:NT + 1], cosC[:].unsqueeze(1), sinC[:].unsqueeze(1))
            release(m_tmp)
            dump("cosT", cosT[:], [r_tab])
            dump("sinC", sinC[:], [r_tab])

            try:
                for l in range(nlayers):
                    layer(b, l, cosT, sinT, cosC, sinC, r_tab)
            except _Stop:
                break
            release(m_tab)

        fw.barrier()
        fw.build()
    print('SBUF high-water', ar['hi'])
    return nc


_NC_CACHE = {}


def kernel(**inputs):
    n_cores = 8
    nseq = 2
    if "nc" not in _NC_CACHE:
        _NC_CACHE["nc"] = build_program(nseq=nseq, nlayers=2)
    nc = _NC_CACHE["nc"]
    x = np.ascontiguousarray(np.asarray(inputs["x"], dtype=np.float32))
    c = np.ascontiguousarray(np.asarray(inputs["c"], dtype=np.float32))
    pos = np.ascontiguousarray(np.asarray(inputs["positions"]).astype(np.int32))
    shared = {n: np.ascontiguousarray(np.asarray(inputs[n], dtype=np.float32)) for n, _ in WEIGHT_SPECS}
    shared["cf"] = _CF
    shared["cb"] = _CB
    in_maps = []
    for k in range(n_cores):
        m = dict(shared)
        m["x"] = x[k * nseq:(k + 1) * nseq]
        m["c"] = c[k * nseq:(k + 1) * nseq]
        m["positions"] = pos[k * nseq:(k + 1) * nseq]
        in_maps.append(m)
    res = run_bass_kernel_spmd(nc, in_maps, core_ids=list(range(n_cores)))
    out = np.concatenate([np.asarray(r["out"], dtype=np.float32) for r in res.results], axis=0)
    return out
```
